# Optimizing a Trainium2 kernel written in Bass

```python
import math
import jax, jax.numpy as jnp
from jax import lax
import numpy as np

D_MODEL = 2048
BATCH = 2
SEQ = 8192
DEPTH = 1

HEAD_DIM = 64
A_Q_HEADS = 16
A_KV_HEADS = 4
A_GROUP = A_Q_HEADS // A_KV_HEADS
A_WINDOW = 128
A_WIDTH = A_Q_HEADS * HEAD_DIM
A_KV_WIDTH = A_KV_HEADS * HEAD_DIM
B_HEADS = 12
B_WIDTH = B_HEADS * HEAD_DIM
B_PATTERNS = ((128, 1), (512, 4), (2048, 16))
BLOCK = 128
IN_SPLITS = (A_WIDTH, A_WIDTH + A_KV_WIDTH, A_WIDTH + 2 * A_KV_WIDTH,
             A_WIDTH + 2 * A_KV_WIDTH + B_WIDTH, A_WIDTH + 2 * A_KV_WIDTH + 2 * B_WIDTH)
IN_COLS = A_WIDTH + 2 * A_KV_WIDTH + 3 * B_WIDTH
N_REL_HEADS = A_Q_HEADS + B_HEADS
REL_BUCKETS = 32
REL_MAX_DIST = 2048
X_HEADS = 4
X_HEAD_DIM = 128
X_WIDTH = X_HEADS * X_HEAD_DIM
MEM_LEN = 256
N_GROUPS = 4
EXP_PER_GROUP = 8
N_EXPERTS = N_GROUPS * EXP_PER_GROUP
TOP_K = 2
D_FF_EXPERT = 512
MOE_BLOCK = 128
EPS = 1e-6
NEG = -1e30

kernel_name = "hybrid_gated_swa_dilated_hmoe"


def rmsnorm(x, g):
    xf = x.astype(jnp.float32)
    y = xf * lax.rsqrt(jnp.mean(xf * xf, axis=-1, keepdims=True) + EPS)
    return (y * g.astype(jnp.float32)).astype(x.dtype)


def t5_bucket(dist):
    max_exact = REL_BUCKETS // 2
    d = jnp.maximum(dist, 0)
    df = jnp.maximum(d, 1).astype(jnp.float32)
    large = max_exact + (jnp.log(df / max_exact) / math.log(REL_MAX_DIST / max_exact)
                         * (REL_BUCKETS - max_exact)).astype(jnp.int32)
    large = jnp.minimum(large, REL_BUCKETS - 1)
    return jnp.where(d < max_exact, d, large)


def banded_attention(q, k, v, bias_table, max_off, step, sinks=None):
    bt, n, hk, g, dh = q.shape
    nb = n // BLOCK
    qb = q.reshape(bt, nb, BLOCK, hk, g, dh)
    pad = ((0, 0), (BLOCK, 0), (0, 0), (0, 0))
    kb = jnp.pad(k, pad).reshape(bt, nb + 1, BLOCK, hk, dh)
    vb = jnp.pad(v, pad).reshape(bt, nb + 1, BLOCK, hk, dh)
    kk = jnp.concatenate([kb[:, :-1], kb[:, 1:]], axis=2)
    vv = jnp.concatenate([vb[:, :-1], vb[:, 1:]], axis=2)
    qi = jnp.arange(BLOCK)[:, None]
    ki = jnp.arange(2 * BLOCK)[None, :]
    dist = qi + BLOCK - ki
    bias = bias_table[t5_bucket(dist * step)]
    bias = jnp.transpose(bias, (2, 3, 0, 1)).astype(jnp.float32)
    key_abs = jnp.arange(nb)[:, None] * BLOCK + ki - BLOCK
    mask = ((dist >= 0) & (dist <= max_off))[None] & (key_abs >= 0)[:, None, :]
    s = jnp.einsum('bnqhgd,bnkhd->bnhgqk', qb, kk).astype(jnp.float32) + bias
    s = jnp.where(mask[None, :, None, None], s, NEG)
    m = jnp.max(s, axis=-1, keepdims=True)
    if sinks is not None:
        sk = sinks.astype(jnp.float32)[None, None, :, :, None, None]
        m = jnp.maximum(m, sk)
    p = jnp.exp(s - m)
    l = jnp.sum(p, axis=-1, keepdims=True)
    if sinks is not None:
        l = l + jnp.exp(sk - m)
    o = jnp.einsum('bnhgqk,bnkhd->bnqhgd', (p / l).astype(v.dtype), vv)
    lse = jnp.transpose((m + jnp.log(l))[..., 0], (0, 1, 4, 2, 3))
    return o.reshape(bt, n, hk, g, dh), lse.reshape(bt, n, hk, g)


def dilated_attention(q, k, v, bias_table):
    b, s, h, dh = q.shape
    outs, lses = [], []
    for w, r in B_PATTERNS:
        span = r * BLOCK
        s_pad = -(-s // span) * span
        n = s_pad // r

        def to_sub(t):
            t = jnp.pad(t, ((0, 0), (0, s_pad - s), (0, 0), (0, 0))).reshape(b, n, r, h, dh)
            return jnp.transpose(t, (0, 2, 1, 3, 4)).reshape(b * r, n, h, dh)

        qs, ks, vs = to_sub(q), to_sub(k), to_sub(v)
        o, lse = banded_attention(qs[:, :, :, None], ks, vs, bias_table[:, :, None], w // r, r)
        o = jnp.transpose(o.reshape(b, r, n, h, dh), (0, 2, 1, 3, 4)).reshape(b, s_pad, h, dh)[:, :s]
        lse = jnp.transpose(lse.reshape(b, r, n, h), (0, 2, 1, 3)).reshape(b, s_pad, h)[:, :s]
        outs.append(o)
        lses.append(lse)
    wts = jax.nn.softmax(jnp.stack(lses, axis=0), axis=0)
    o = jnp.sum(wts[..., None] * jnp.stack(outs, axis=0).astype(jnp.float32), axis=0)
    return o.astype(q.dtype)


def cross_attention(h, mem_n, w_xq, w_xk, w_xv, w_xo):
    b, s, _ = h.shape
    ml = mem_n.shape[1]
    q = (h @ w_xq).reshape(b, s, X_HEADS, X_HEAD_DIM) * (X_HEAD_DIM ** -0.5)
    k = (mem_n @ w_xk).reshape(b, ml, X_HEADS, X_HEAD_DIM)
    v = (mem_n @ w_xv).reshape(b, ml, X_HEADS, X_HEAD_DIM)
    p = jax.nn.softmax(jnp.einsum('bshd,bmhd->bhsm', q, k).astype(jnp.float32), axis=-1)
    o = jnp.einsum('bhsm,bmhd->bshd', p.astype(v.dtype), v).reshape(b, s, X_WIDTH)
    return o @ w_xo


def hier_moe(h, w_rg, w_re, w1, w3, w2):
    b, s, d = h.shape
    t = b * s
    hf = h.reshape(t, d)
    g_logits = (hf @ w_rg).astype(jnp.float32)
    g_prob = jax.nn.softmax(g_logits, axis=-1)
    g_gate, g_idx = lax.top_k(g_prob, 1)
    e_logits = (hf @ w_re).astype(jnp.float32).reshape(t, N_GROUPS, EXP_PER_GROUP)
    e_logits = jnp.take_along_axis(e_logits, g_idx[:, :, None], axis=1)[:, 0]
    top_v, top_i = lax.top_k(e_logits, TOP_K)
    gate = jax.nn.softmax(top_v, axis=-1) * g_gate
    expert = g_idx * EXP_PER_GROUP + top_i

    n_assign = t * TOP_K
    e_flat = expert.reshape(-1)
    tok_flat = jnp.broadcast_to(jnp.arange(t, dtype=jnp.int32)[:, None], (t, TOP_K)).reshape(-1)
    w_flat = gate.reshape(-1)
    order = jnp.argsort(e_flat)
    e_sorted = e_flat[order]
    counts = jnp.bincount(e_flat, length=N_EXPERTS)
    padded = (counts + MOE_BLOCK - 1) // MOE_BLOCK * MOE_BLOCK
    start = jnp.cumsum(counts) - counts
    pend = jnp.cumsum(padded)
    pstart = pend - padded
    dest = pstart[e_sorted] + (jnp.arange(n_assign) - start[e_sorted])
    n_rows = -(-(n_assign + N_EXPERTS * (MOE_BLOCK - 1)) // MOE_BLOCK) * MOE_BLOCK
    n_blocks = n_rows // MOE_BLOCK
    row_tok = jnp.full((n_rows,), t, jnp.int32).at[dest].set(tok_flat[order])
    row_w = jnp.zeros((n_rows,), jnp.float32).at[dest].set(w_flat[order])
    block_e = jnp.minimum(jnp.searchsorted(pend, jnp.arange(n_blocks) * MOE_BLOCK, side='right'),
                          N_EXPERTS - 1)
    h_pad = jnp.concatenate([hf, jnp.zeros((1, d), hf.dtype)], axis=0)
    xb = h_pad[row_tok].reshape(n_blocks, MOE_BLOCK, d)

    def expert_block(args):
        xk, e = args
        return (jax.nn.silu(xk @ w1[e]) * (xk @ w3[e])) @ w2[e]

    yb = lax.map(expert_block, (xb, block_e)).reshape(n_rows, d)
    y = jnp.zeros((t + 1, d), jnp.float32).at[row_tok].add(yb.astype(jnp.float32) * row_w[:, None])
    return y[:t].reshape(b, s, d).astype(h.dtype)


def setup_inputs(seed: int = 0) -> dict:
    key = jax.random.key(seed)
    ks = jax.random.split(key, 24)
    f32 = jnp.float32
    L = DEPTH

    def nrm(k, shape, scale):
        return jax.random.normal(k, shape, f32) * scale

    return {
        "x": nrm(ks[0], (BATCH, SEQ, D_MODEL), 1.0),
        "mem": nrm(ks[1], (BATCH, MEM_LEN, D_MODEL), 1.0),
        "rel_bias": nrm(ks[2], (REL_BUCKETS, N_REL_HEADS), 0.5),
        "g_mix": 1.0 + nrm(ks[3], (L, D_MODEL), 0.02),
        "w_in": nrm(ks[4], (L, D_MODEL, IN_COLS), D_MODEL ** -0.5),
        "sinks_a": nrm(ks[5], (L, A_Q_HEADS), 0.5),
        "w_a_out": nrm(ks[6], (L, A_WIDTH, D_MODEL), A_WIDTH ** -0.5),
        "w_b_out": nrm(ks[7], (L, B_WIDTH, D_MODEL), B_WIDTH ** -0.5),
        "w_gate": nrm(ks[8], (L, D_MODEL, 2 * D_MODEL), D_MODEL ** -0.5),
        "b_gate": nrm(ks[9], (L, 2 * D_MODEL), 0.1),
        "w_o": nrm(ks[10], (L, D_MODEL, D_MODEL), D_MODEL ** -0.5),
        "g_x": 1.0 + nrm(ks[11], (L, D_MODEL), 0.02),
        "g_mem": 1.0 + nrm(ks[12], (L, D_MODEL), 0.02),
        "w_xq": nrm(ks[13], (L, D_MODEL, X_WIDTH), D_MODEL ** -0.5),
        "w_xk": nrm(ks[14], (L, D_MODEL, X_WIDTH), D_MODEL ** -0.5),
        "w_xv": nrm(ks[15], (L, D_MODEL, X_WIDTH), D_MODEL ** -0.5),
        "w_xo": nrm(ks[16], (L, X_WIDTH, D_MODEL), X_WIDTH ** -0.5),
        "g_moe": 1.0 + nrm(ks[17], (L, D_MODEL), 0.02),
        "w_rg": nrm(ks[18], (L, D_MODEL, N_GROUPS), D_MODEL ** -0.5),
        "w_re": nrm(ks[19], (L, D_MODEL, N_EXPERTS), D_MODEL ** -0.5),
        "w1": nrm(ks[20], (L, N_EXPERTS, D_MODEL, D_FF_EXPERT), D_MODEL ** -0.5),
        "w3": nrm(ks[21], (L, N_EXPERTS, D_MODEL, D_FF_EXPERT), D_MODEL ** -0.5),
        "w2": nrm(ks[22], (L, N_EXPERTS, D_FF_EXPERT, D_MODEL), D_FF_EXPERT ** -0.5),
        "g_final": 1.0 + nrm(ks[23], (D_MODEL,), 0.02),
    }


def reference(x, mem, rel_bias, g_mix, w_in, sinks_a, w_a_out, w_b_out, w_gate, b_gate, w_o,
              g_x, g_mem, w_xq, w_xk, w_xv, w_xo, g_moe, w_rg, w_re, w1, w3, w2, g_final):
    b, s, d = x.shape
    scale = HEAD_DIM ** -0.5
    bias_a = rel_bias[:, :A_Q_HEADS].reshape(REL_BUCKETS, A_KV_HEADS, A_GROUP)
    bias_b = rel_bias[:, A_Q_HEADS:]
    for l in range(DEPTH):
        h = rmsnorm(x, g_mix[l])
        qa, ka, va, qb, kb, vb = jnp.split(h @ w_in[l], IN_SPLITS, axis=-1)
        qa = qa.reshape(b, s, A_KV_HEADS, A_GROUP, HEAD_DIM) * scale
        ka = ka.reshape(b, s, A_KV_HEADS, HEAD_DIM)
        va = va.reshape(b, s, A_KV_HEADS, HEAD_DIM)
        oa, _ = banded_attention(qa, ka, va, bias_a, A_WINDOW - 1, 1,
                                 sinks=sinks_a[l].reshape(A_KV_HEADS, A_GROUP))
        ya = oa.reshape(b, s, A_WIDTH) @ w_a_out[l]
        ob = dilated_attention(qb.reshape(b, s, B_HEADS, HEAD_DIM) * scale,
                               kb.reshape(b, s, B_HEADS, HEAD_DIM),
                               vb.reshape(b, s, B_HEADS, HEAD_DIM), bias_b)
        yb = ob.reshape(b, s, B_WIDTH) @ w_b_out[l]
        gates = jax.nn.sigmoid((h @ w_gate[l] + b_gate[l]).astype(jnp.float32)).astype(x.dtype)
        ga, gb = gates[..., :D_MODEL], gates[..., D_MODEL:]
        x = x + (ga * ya + gb * yb) @ w_o[l]
        x = x + cross_attention(rmsnorm(x, g_x[l]), rmsnorm(mem, g_mem[l]),
                                w_xq[l], w_xk[l], w_xv[l], w_xo[l])
        x = x + hier_moe(rmsnorm(x, g_moe[l]), w_rg[l], w_re[l], w1[l], w3[l], w2[l])
    return rmsnorm(x, g_final)
```

```python
import math
from contextlib import ExitStack

import numpy as np
import concourse.bass as bass
import concourse.mybir as mybir
from concourse.bass_utils import run_bass_kernel_spmd

F32 = mybir.dt.float32
BF16 = mybir.dt.bfloat16
I32 = mybir.dt.int32
AF = mybir.ActivationFunctionType
ALU = mybir.AluOpType
AX = mybir.AxisListType

D = 2048
T = 2048
NT = 16
NST = 4
KC = 16
NCORES = 8
CAP = 256
NEXP = 32
NROWS = NEXP * CAP + 128
EPS = 1e-6
NEGM = -30000.0


class Sched:
    def __init__(self, nc, es):
        self.nc = nc
        self.es = es
        self.eng = dict(pe=nc.tensor, act=nc.scalar, dve=nc.vector, pool=nc.gpsimd, sp=nc.sync)
        self.sem = {k: es.enter_context(nc.semaphore("s_" + k)) for k in self.eng}
        self.cnt = {k: 0 for k in self.eng}
        self.seen = {k: {} for k in self.eng}
        self.dsem = {}

    def wait(self, e, deps):
        for d in deps:
            if d is None:
                continue
            if isinstance(d, list):
                self.wait(e, d)
                continue
            kind, key, val = d
            if kind == 'e' and key == e:
                continue
            sk = (kind, key)
            if self.seen[e].get(sk, 0) >= val:
                continue
            sem = self.sem[key] if kind == 'e' else self.dsem[key][0]
            self.eng[e].wait_ge(sem, val)
            self.seen[e][sk] = val

    def op(self, e, fn, deps=(), sig=True, ss=False):
        self.wait(e, deps)
        if ss and self.cnt[e] > 0 and self.seen[e].get(('e', e), 0) < self.cnt[e]:
            self.eng[e].wait_ge(self.sem[e], self.cnt[e])
            self.seen[e][('e', e)] = self.cnt[e]
        ins = fn(self.eng[e])
        if sig:
            self.cnt[e] += 1
            ins.then_inc(self.sem[e], 1)
            return ('e', e, self.cnt[e])
        return ('e', e, self.cnt[e] + 1)

    def dma(self, q, out, in_, deps=(), stream="d", **kw):
        self.wait(q, deps)
        for d in deps:
            if isinstance(d, tuple) and d[0] == 'e' and d[1] == q and self.seen[q].get(('e', q), 0) < d[2]:
                self.eng[q].wait_ge(self.sem[q], d[2])
                self.seen[q][('e', q)] = d[2]
        if stream not in self.dsem:
            self.dsem[stream] = [self.es.enter_context(self.nc.semaphore("d_" + stream)), 0]
        s = self.dsem[stream]
        s[1] += 16
        self.eng[q].dma_start(out=out, in_=in_, **kw).then_inc(s[0], 16)
        return ('d', stream, s[1])

    def idma(self, out, out_offset, in_, in_offset, deps=(), stream="i", **kw):
        q = 'pool'
        self.wait(q, deps)
        if isinstance(kw.get("bounds_check"), int):
            if not hasattr(self, "_bc"):
                self._bc = {}
            v = kw["bounds_check"]
            if v not in self._bc:
                self._bc[v] = self.eng[q].to_reg(v)
            kw["bounds_check"] = self._bc[v]
        if stream not in self.dsem:
            self.dsem[stream] = [self.es.enter_context(self.nc.semaphore("d_" + stream)), 0]
        s = self.dsem[stream]
        s[1] += 16
        self.eng[q].indirect_dma_start(out=out, out_offset=out_offset, in_=in_, in_offset=in_offset,
                                       **kw).then_inc(s[0], 16)
        return ('d', stream, s[1])

    def all_tokens(self, skip=()):
        toks = [('e', k, v) for k, v in self.cnt.items() if v > 0]
        toks += [('d', k, v[1]) for k, v in self.dsem.items() if v[1] > 0 and k not in skip]
        return toks

    def barrier(self, skip=("bg",)):
        toks = self.all_tokens(skip)
        for e in self.eng:
            self.wait(e, toks)


class Ring:
    def __init__(self, tiles):
        self.tiles = tiles
        self.war = [[] for _ in tiles]
        self.i = -1

    def next(self):
        self.i = (self.i + 1) % len(self.tiles)
        return self.i, self.tiles[self.i], self.war[self.i]

    def done(self, i, toks):
        self.war[i] = [t for t in toks if t is not None]


def alloc(nc, es, name, shape, dt, n=None):
    if n is None:
        return es.enter_context(nc.sbuf_tensor(name, shape, dt))
    return Ring([es.enter_context(nc.sbuf_tensor(f"{name}{i}", shape, dt)) for i in range(n)])


def build(phases=99, taps=()):
    nc = bass.Bass("TRN2", target_bir_lowering=False)

    def din(name, shape, dt=F32):
        return nc.dram_tensor(name, list(shape), dt, kind="ExternalInput").ap()

    def dscr(name, shape, dt, tap=False):
        kind = "ExternalOutput" if name in taps else "Internal"
        return nc.dram_tensor(name, list(shape), dt, kind=kind).ap()

    xo = din("xo", [T, D])
    xh = din("xh", [T, D])
    hvalid = din("hvalid", [128, 64])
    g_mix = din("g_mix", [1, D])
    w_in = din("w_in", [D, 3840])
    biasA = din("biasA", [128, 4, 2, 2, 2, 128])
    biasB = din("biasB", [128, 3, 12, 2, 128])
    maskA = din("maskA", [128, 2, 128])
    maskB = din("maskB", [128, 2, 128])
    sinks = din("sinks", [1, 16])
    if phases >= 3:
        w_a_out = din("w_a_out", [1024, D])
        w_b_out = din("w_b_out", [768, D])
        w_gate = din("w_gate", [D, 2 * D])
        b_gate = din("b_gate", [128, 32])
    if phases >= 4:
        w_o = din("w_o", [D, D])
    if phases >= 5:
        memb = din("memb", [256, D])
        g_x = din("g_x", [1, D])
        g_mem = din("g_mem", [1, D])
        g_moe = din("g_moe", [1, D])
        w_xq = din("w_xq", [D, 512])
        w_xk = din("w_xk", [D, 512])
        w_xv = din("w_xv", [D, 512])
        w_xo = din("w_xo", [512, D])
        w_r = din("w_r", [D, 36])
    if phases >= 6:
        w1 = din("w1", [NEXP, D, 512])
        w3 = din("w3", [NEXP, D, 512])
        w2 = din("w2", [NEXP, 512, D])
    if phases >= 7:
        g_final = din("g_final", [1, D])
        out = nc.dram_tensor("out", [T, D], F32, kind="ExternalOutput").ap()

    hT_scr = dscr("hT_scr", [128, KC, 2 * T], BF16)
    QT_scr = dscr("QT_scr", [14, 128, T], BF16)
    KT_scr = dscr("KT_scr", [8, 128, 2 * T], BF16)
    V_scr = dscr("V_scr", [2 * T, 1024], BF16)
    OT_scr = dscr("OT_scr", [14, 128, T], BF16)
    zT_scr = dscr("zT_scr", [KC, 128, T], BF16)
    x1_scr = dscr("x1_scr", [T, D], F32)
    x2_scr = dscr("x2_scr", [T, D], F32)
    X_disp = dscr("X_disp", [NROWS, D], BF16)
    Y_disp = dscr("Y_disp", [NROWS, D], F32)
    route_scr = dscr("route_scr", [128, 4, NT], F32)
    if phases >= 6:
        w1b_scr = dscr("w1b_scr", [NEXP, 128, KC, 512], BF16)
        w3b_scr = dscr("w3b_scr", [NEXP, 128, KC, 512], BF16)
        w2b_scr = dscr("w2b_scr", [NEXP, 128, 4, D], BF16)

    with ExitStack() as es:
        S = Sched(nc, es)
        ps = [es.enter_context(nc.psum_tensor(f"ps{i}", [128, 512], F32)) for i in range(8)]
        psr = Ring(ps)
        block = es.enter_context(nc.Block())

        @block.gpsimd
        def _(_g):
            identb = alloc(nc, es, "identb", [128, 128], BF16)
            identf = alloc(nc, es, "identf", [128, 128], F32)
            ones_b = alloc(nc, es, "ones_b", [128, 128], BF16)
            onesh_b = alloc(nc, es, "onesh_b", [128, 64], BF16)
            hv_f = alloc(nc, es, "hv_f", [128, 64], F32)
            eps_t = alloc(nc, es, "eps_t", [128, 1], F32)
            t_c = []
            t_c.append(S.op('pool', lambda e: e.memset(identf[:], 0.0)))
            t_c.append(S.op('pool', lambda e: e.affine_select(
                out=identf[:], in_=identf[:], pattern=[[-1, 128]], base=0, channel_multiplier=1,
                compare_op=ALU.not_equal, fill=1.0)))
            t_c.append(S.op('pool', lambda e: e.tensor_copy(out=identb[:], in_=identf[:])))
            t_c.append(S.op('pool', lambda e: e.memset(ones_b[:], 1.0)))
            t_c.append(S.op('pool', lambda e: e.memset(eps_t[:], EPS)))
            t = S.dma('sp', hv_f[:], hvalid, stream="c0")
            t_c.append(S.op('dve', lambda e: e.tensor_copy(out=onesh_b[:], in_=hv_f[:]), deps=[t]))
            C = dict(identb=identb, identf=identf, ones_b=ones_b, onesh_b=onesh_b, eps_t=eps_t)
            S.barrier()

            bg_list = []
            if phases >= 6:
                for ex_ in range(NEXP):
                    bg_list += [(w1b_scr[ex_], w1[ex_].rearrange("(c p) n -> p c n", p=128)),
                                (w3b_scr[ex_], w3[ex_].rearrange("(c p) n -> p c n", p=128)),
                                (w2b_scr[ex_], w2[ex_].rearrange("(c p) n -> p c n", p=128))]
            bg_state = [0]

            def bgc(n=1):
                for _ in range(n):
                    if bg_state[0] < len(bg_list):
                        o_, i_ = bg_list[bg_state[0]]
                        bg_state[0] += 1
                        S.dma('pool', o_, i_, stream="bg")

            def rmsnorm_tile(xt, g_bc, hb_out, ss_col, tmp_col, rstd_col, junk, deps):
                t1 = S.op('act', lambda e: e.activation(out=junk[:], in_=xt, func=AF.Square,
                                                        accum_out=ss_col), deps=deps)
                t2 = S.op('act', lambda e: e.activation(out=tmp_col, in_=ss_col, func=AF.Sqrt,
                                                        bias=eps_t[:], scale=1.0 / D), deps=[t1], ss=True)
                t3 = S.op('dve', lambda e: e.reciprocal(out=rstd_col, in_=tmp_col), deps=[t2])
                t4 = S.op('dve', lambda e: e.scalar_tensor_tensor(
                    out=hb_out, in0=xt, scalar=rstd_col, in1=g_bc, op0=ALU.mult, op1=ALU.mult),
                    deps=[t3] + list(deps), ss=True)
                return t4, t1

            def transpose_tile(hb, dstT, deps, war_dst):
                toks = []
                for half in range(2):
                    bi, bank, war = psr.next()
                    pv = bank[:].bitcast(BF16)
                    tp = None
                    for j in range(8):
                        c = half * 8 + j
                        tp = S.op('pe', lambda e, c=c, j=j: e.transpose(
                            out=pv[:, j * 128:(j + 1) * 128], in_=hb[:, c * 128:(c + 1) * 128],
                            identity=identb[:]), deps=list(deps) + war if j == 0 else (), sig=(j == 7))
                    eng = 'act' if half == 0 else 'dve'
                    src = pv.rearrange("p (c t) -> p c t", t=128)
                    dst = dstT[:, half * 8:(half + 1) * 8, :]
                    if eng == 'act':
                        tc_ = S.op('act', lambda e: e.activation(out=dst, in_=src, func=AF.Copy),
                                   deps=[tp] + war_dst)
                    else:
                        tc_ = S.op('dve', lambda e: e.tensor_copy(out=dst, in_=src), deps=[tp] + war_dst)
                    psr.done(bi, [tc_])
                    toks.append(tc_)
                return toks

            with ExitStack() as pes:
                g_bc = alloc(nc, pes, "g_bc", [128, D], F32)
                tg = S.dma('sp', g_bc[:], g_mix.partition_broadcast(128), stream="c0")
                xr = alloc(nc, pes, "xr", [128, D], F32, 3)
                hbr = alloc(nc, pes, "hbr", [128, D], BF16, 3)
                junk = alloc(nc, pes, "junk", [128, D], BF16)
                hTr = alloc(nc, pes, "hTr", [128, KC, 512], BF16, 2)
                ss = alloc(nc, pes, "ss", [128, 32], F32)
                tmpc = alloc(nc, pes, "tmpc", [128, 32], F32)
                rstd = alloc(nc, pes, "rstd", [128, 32], F32)
                def p1_s1(t):
                    src_ = xh if t < 16 else xo
                    r0 = (t % 16) * 128
                    xi, xt, xwar = xr.next()
                    tl = S.dma('sp', xt[:], src_[r0:r0 + 128, :], deps=xwar, stream=f"x{xi}")
                    bi, hb, bwar = hbr.next()
                    t4, t1 = rmsnorm_tile(xt[:], g_bc[:], hb[:], ss[:, t:t + 1], tmpc[:, t:t + 1],
                                          rstd[:, t:t + 1], junk, [tl, tg] + bwar)
                    xr.done(xi, [t4, t1])
                    return bi, hb, t4

                cur = {}

                def p1_s2(t, st_):
                    bi, hb, t4 = st_
                    st, tt = t // 4, t % 4
                    if tt == 0:
                        hi, hT, hwar = hTr.next()
                        cur.update(hi=hi, hT=hT, hwar=hwar, tcs=[])
                    tks = transpose_tile(hb, cur["hT"][:, :, tt * 128:(tt + 1) * 128], [t4], cur["hwar"])
                    hbr.done(bi, tks)
                    cur["tcs"] += tks
                    if tt == 3:
                        td = S.dma('act', hT_scr[:, :, st * 512:(st + 1) * 512], cur["hT"][:], deps=cur["tcs"], stream=f"h{cur['hi']}")
                        hTr.done(cur["hi"], [td])

                run_pipeline2 = None
                stt_ = {}
                for t in range(33):
                    if t < 32:
                        stt_[t] = p1_s1(t)
                    if t >= 1:
                        p1_s2(t - 1, stt_.pop(t - 1))
                S.barrier()
            if phases <= 1:
                return

            with ExitStack() as pes:
                wq = alloc(nc, pes, "wq", [128, KC, 1792], BF16)
                wkv = alloc(nc, pes, "wkv", [128, KC, 2048], BF16)
                w_in_v = w_in.rearrange("(c p) n -> p c n", p=128)
                tw = []
                for c4 in range(4):
                    tw.append(S.dma('pool', wq[:, c4 * 4:(c4 + 1) * 4, 0:1024], w_in_v[:, c4 * 4:(c4 + 1) * 4, 0:1024],
                                    stream="w0"))
                    tw.append(S.dma('pool', wq[:, c4 * 4:(c4 + 1) * 4, 1024:1792],
                                    w_in_v[:, c4 * 4:(c4 + 1) * 4, 1536:2304], stream="w0"))
                tw = [tw[-1]]
                twkv = []
                for c4 in range(4):
                    twkv.append(S.dma('pool', wkv[:, c4 * 4:(c4 + 1) * 4, 0:512], w_in_v[:, c4 * 4:(c4 + 1) * 4, 1024:1536],
                                      stream="w1"))
                    twkv.append(S.dma('pool', wkv[:, c4 * 4:(c4 + 1) * 4, 512:2048],
                                      w_in_v[:, c4 * 4:(c4 + 1) * 4, 2304:3840], stream="w1"))
                twkv = [twkv[-1]]
                hTr = alloc(nc, pes, "hTq", [128, KC, 512], BF16, 2)
                qsr = alloc(nc, pes, "qsr", [128, 512], BF16, 4)
                for st in range(4):
                    hi, hT, hwar = hTr.next()
                    tl = S.dma('sp', hT[:], hT_scr[:, :, T + st * 512:T + (st + 1) * 512], deps=hwar, stream=f"h{hi}")
                    last = []
                    for fb in range(14):
                        bi, bank, war = psr.next()
                        tm = None
                        for c in range(KC):
                            tm = S.op('pe', lambda e, c=c: e.matmul(
                                bank[:], lhsT=wq[:, c, fb * 128:(fb + 1) * 128], rhs=hT[:, c, :],
                                start=(c == 0), stop=(c == KC - 1)),
                                deps=[tl] + tw + war if c == 0 else (), sig=(c == KC - 1))
                        qi, qs, qwar = qsr.next()
                        te = S.op('act', lambda e: e.activation(out=qs[:], in_=bank[:], func=AF.Copy, scale=0.125),
                                  deps=[tm] + qwar)
                        psr.done(bi, [te])
                        td = S.dma('act', QT_scr[fb, :, st * 512:(st + 1) * 512], qs[:], deps=[te], stream=f"q{qi}")
                        qsr.done(qi, [td])
                        last = [tm]
                    hTr.done(hi, last)
                    bgc(3)
                tw = twkv
                ksr = alloc(nc, pes, "ksr", [128, 512], BF16, 4)
                vsr = alloc(nc, pes, "vsr", [128, 1024], BF16, 2)
                flip = 0
                for st in range(8):
                    hi, hT, hwar = hTr.next()
                    tl = S.dma('sp', hT[:], hT_scr[:, :, st * 512:(st + 1) * 512], deps=hwar, stream=f"h{hi}")
                    last = []
                    for kb in range(8):
                        if kb < 2 and st < 3:
                            continue
                        col = kb * 128 if kb < 2 else 512 + (kb - 2) * 128
                        bi, bank, war = psr.next()
                        tm = None
                        for c in range(KC):
                            tm = S.op('pe', lambda e, c=c: e.matmul(
                                bank[:], lhsT=wkv[:, c, col:col + 128], rhs=hT[:, c, :],
                                start=(c == 0), stop=(c == KC - 1)),
                                deps=[tl] + tw + war if c == 0 else (), sig=(c == KC - 1))
                        ki, ks, kwar = ksr.next()
                        flip ^= 1
                        if flip:
                            te = S.op('act', lambda e: e.activation(out=ks[:], in_=bank[:], func=AF.Copy),
                                      deps=[tm] + kwar)
                        else:
                            te = S.op('dve', lambda e: e.tensor_copy(out=ks[:], in_=bank[:]), deps=[tm] + kwar)
                        psr.done(bi, [te])
                        td = S.dma('act', KT_scr[kb, :, st * 512:(st + 1) * 512], ks[:], deps=[te], stream=f"k{ki}")
                        ksr.done(ki, [td])
                        last = [tm]
                    for tt in range(4):
                        vi, vs, vwar = vsr.next()
                        tes = []
                        segs = [(256, 512, 1280), (768, 256, 1792)]
                        if st >= 3:
                            segs = [(0, 256, 256)] + segs
                        else:
                            tes.append(S.op('pool', lambda e: e.memset(vs[:, 0:256], 0.0), deps=vwar))
                        for (d0, wdt, s0) in segs:
                            bi, bank, war = psr.next()
                            tm = None
                            for c in range(KC):
                                tm = S.op('pe', lambda e, c=c: e.matmul(
                                    bank[:, 0:wdt], lhsT=hT[:, c, tt * 128:(tt + 1) * 128], rhs=wkv[:, c, s0:s0 + wdt],
                                    start=(c == 0), stop=(c == KC - 1)),
                                    deps=[tl] + tw + war if c == 0 else (), sig=(c == KC - 1))
                            flip ^= 1
                            if flip:
                                te = S.op('act', lambda e: e.activation(out=vs[:, d0:d0 + wdt], in_=bank[:, 0:wdt],
                                                                        func=AF.Copy), deps=[tm] + vwar)
                            else:
                                te = S.op('dve', lambda e: e.tensor_copy(out=vs[:, d0:d0 + wdt], in_=bank[:, 0:wdt]),
                                          deps=[tm] + vwar)
                            psr.done(bi, [te])
                            tes.append(te)
                            last = [tm]
                        r0 = st * 512 + tt * 128
                        td = S.dma('act', V_scr[r0:r0 + 128, :], vs[:], deps=tes, stream=f"v{vi}")
                        vsr.done(vi, [td])
                    hTr.done(hi, last)
                    bgc(2 if st < 7 else 4)
                S.barrier()
            if phases <= 2:
                return

            def sl(start, n, step):
                return slice(start, start + (n - 1) * step + 1, step)

            def run_pipeline(n, stage1, stage2):
                stt = {}
                for b_ in range(n + 1):
                    if b_ < n:
                        stt[b_] = stage1(b_)
                    if b_ >= 1:
                        stage2(b_ - 1, stt.pop(b_ - 1))

            with ExitStack() as pes:
                bA_b = alloc(nc, pes, "bA_b", [128, 16, 2, 128], BF16)
                bB_b = alloc(nc, pes, "bB_b", [128, 36, 2, 2, 128], BF16)
                esk = alloc(nc, pes, "esk", [64, 16], F32)
                with ExitStack() as tes_:
                    bA = alloc(nc, tes_, "bA", [128, 16, 2, 128], F32)
                    bB = alloc(nc, tes_, "bB", [128, 36, 2, 128], F32)
                    mA = alloc(nc, tes_, "mA", [128, 2, 128], F32)
                    mB = alloc(nc, tes_, "mB", [128, 2, 128], F32)
                    sk = alloc(nc, tes_, "sk", [64, 16], F32)
                    t0 = S.dma('sp', bA[:], biasA.rearrange("k g p j w q -> k (g p j) w q"), stream="c0")
                    t0 = S.dma('sp', bB[:], biasB.rearrange("k p h w q -> k (p h) w q"), stream="c0")
                    t0 = S.dma('sp', mA[:], maskA, stream="c0")
                    t0 = S.dma('sp', mB[:], maskB, stream="c0")
                    t0 = S.dma('sp', sk[:], sinks.partition_broadcast(64), stream="c0")
                    S.op('dve', lambda e: e.tensor_tensor(out=bA_b[:], in0=bA[:], in1=mA[:].unsqueeze(1).to_broadcast([128, 16, 2, 128]),
                                                          op=ALU.add), deps=[t0])
                    for u in range(2):
                        S.op('dve', lambda e: e.tensor_tensor(out=bB_b[:, :, u], in0=bB[:],
                                                              in1=mB[:].unsqueeze(1).to_broadcast([128, 36, 2, 128]), op=ALU.add))
                    S.op('act', lambda e: e.activation(out=esk[:], in_=sk[:], func=AF.Exp), deps=[t0])
                    S.barrier()
                psA = Ring(ps[0:4])
                psB = Ring(ps[4:8])
                pTr = alloc(nc, pes, "pTr", [128, 512], BF16, 6)

                with ExitStack() as aes:
                    kTAr = alloc(nc, aes, "kTA", [128, 17 * 128], BF16, 2)
                    qTAr = alloc(nc, aes, "qTA", [128, 2, 2, T], BF16, 2)
                    for q_ in qTAr.tiles:
                        S.op('pool', lambda e: e.memset(q_[:], 0.0))
                    VAr = alloc(nc, aes, "VA", [128, 17, 64], BF16, 2)
                    oAr = alloc(nc, aes, "oA", [64, 4, T], BF16, 2)
                    dsb = alloc(nc, aes, "dsb", [64, 512], F32, 2)

                    def loadA(g):
                        kb, ko = g // 2, (g % 2) * 64
                        ki, kTA, kwar = kTAr.next()
                        _, qTA, qwar = qTAr.next()
                        _, VA, vwar = VAr.next()
                        tl = None
                        for half in range(2):
                            tl = S.dma('sp', kTA[half * 64:(half + 1) * 64, :], KT_scr[kb, ko:ko + 64, T - 128:2 * T],
                                       deps=kwar + qwar + vwar, stream=f"a{ki}")
                        for par_ in range(2):
                            tl = S.dma('sp', qTA[par_ * 64:(par_ + 1) * 64, par_, :, :],
                                       QT_scr[2 * g:2 * g + 2, par_ * 64:(par_ + 1) * 64, :].rearrange("b p t -> p b t"),
                                       deps=[('e', 'pool', S.cnt['pool'])], stream=f"a{ki}")
                        tl = S.dma('sp', VA[:], V_scr[T - 128:2 * T, g * 64:(g + 1) * 64].rearrange("(i p) c -> p i c", p=128),
                                   stream=f"a{ki}")
                        return ki, kTA, qTA, VA, [tl]

                    nxt = loadA(0)
                    for g in range(4):
                        ki, kTA, qTA, VA, tl = nxt
                        if g + 1 < 4:
                            nxt = loadA(g + 1)
                        oi, oA, owar = oAr.next()
                        lastpe = [None]
                        lastf3 = [None]

                        def a_stage1(i):
                            pts = []
                            for par in range(2):
                                o = par * 64
                                bi, bank, war = psA.next()
                                S.op('pe', lambda e: e.matmul(bank[:], lhsT=identb[:],
                                                              rhs=bA_b[:, g * 4 + par * 2:g * 4 + par * 2 + 2].rearrange("k j w q -> k (j w q)"),
                                                              start=True, stop=False), deps=tl + war, sig=False)
                                tm = None
                                n = 0
                                for jj in range(2):
                                    for w in range(2):
                                        kt = i + w
                                        tm = S.op('pe', lambda e: e.matmul(
                                            bank[:, (jj * 2 + w) * 128:(jj * 2 + w + 1) * 128],
                                            lhsT=kTA[:, kt * 128:(kt + 1) * 128],
                                            rhs=qTA[:, par, jj, i * 128:(i + 1) * 128], start=False, stop=(n == 3)), sig=(n == 3))
                                        n += 1
                                pi, pT, pwar = pTr.next()
                                te = S.op('act', lambda e: e.activation(out=pT[:], in_=bank[:], func=AF.Exp), deps=[tm] + pwar)
                                psA.done(bi, [te])
                                pts.append((pi, pT, te))
                            return pts

                        def a_stage2(i, pts):
                            nbi, nbank, nwar = psB.next()
                            dbi, dbank, dwar = psB.next()
                            tn = td_ = None
                            for par in range(2):
                                pi, pT, te = pts[par]
                                pv = pT[:].rearrange("k (j w q) -> k j w q", j=2, w=2)
                                for w in range(2):
                                    kt = i + w
                                    tn = S.op('pe', lambda e: e.matmul(
                                        nbank[0:64, par * 256:(par + 1) * 256], lhsT=VA[:, kt, :], rhs=pv[:, :, w, :],
                                        start=(w == 0), stop=(w == 1)), deps=[te] + nwar, sig=(par == 1 and w == 1))
                            for par in range(2):
                                pi, pT, te = pts[par]
                                pv = pT[:].rearrange("k (j w q) -> k j w q", j=2, w=2)
                                for w in range(2):
                                    lh = onesh_b[:, :] if (i == 0 and w == 0) else ones_b[:, 0:64]
                                    td_ = S.op('pe', lambda e: e.matmul(
                                        dbank[0:64, par * 256:(par + 1) * 256], lhsT=lh, rhs=pv[:, :, w, :],
                                        start=(w == 0), stop=(w == 1)), deps=[te] + dwar, sig=(par == 1 and w == 1))
                            for par in range(2):
                                pTr.done(pts[par][0], [td_])
                            di, ds, dswar = dsb.next()
                            f1 = S.op('dve', lambda e: e.tensor_tensor(
                                out=ds[:].rearrange("p (h q) -> p h q", h=4), in0=dbank[0:64, :].rearrange("p (h q) -> p h q", h=4),
                                in1=esk[:, g * 4:(g + 1) * 4].unsqueeze(2).to_broadcast([64, 4, 128]), op=ALU.add),
                                deps=[td_] + dswar)
                            f2 = S.op('dve', lambda e: e.reciprocal(out=ds[:], in_=ds[:]))
                            f3 = S.op('dve', lambda e: e.tensor_tensor(
                                out=oA[:, :, i * 128:(i + 1) * 128], in0=nbank[0:64, :].rearrange("p (h q) -> p h q", h=4),
                                in1=ds[:].rearrange("p (h q) -> p h q", h=4), op=ALU.mult), deps=[tn] + (owar if i == 0 else []))
                            psB.done(nbi, [f3])
                            psB.done(dbi, [f1])
                            dsb.done(di, [f3])
                            lastpe[0] = td_
                            lastf3[0] = f3

                        run_pipeline(NT, a_stage1, a_stage2)
                        kTAr.done(ki, [lastpe[0]])
                        qTAr.done(ki, [lastpe[0]])
                        VAr.done(ki, [lastpe[0]])
                        tds = []
                        for par in range(2):
                            for jj in range(2):
                                tds.append(S.dma('pool', OT_scr[2 * g + jj, par * 64:(par + 1) * 64, :], oA[:, par * 2 + jj, :],
                                                 deps=[lastf3[0]], stream=f"o{oi}"))
                        oAr.done(oi, [tds[-1]])
                        bgc(3)
                    S.barrier()

                with ExitStack() as bes:
                    kTBr = alloc(nc, bes, "kTB", [128, 2 * T], BF16, 2)
                    qTBr = alloc(nc, bes, "qTB", [128, 2, T], BF16, 2)
                    for q_ in qTBr.tiles:
                        S.op('pool', lambda e: e.memset(q_[:], 0.0))
                    VBr = alloc(nc, bes, "VB", [128, 32, 128], BF16, 2)
                    accnr = alloc(nc, bes, "accn", [64, 2, T], F32, 2)
                    accdr = alloc(nc, bes, "accd", [64, 2, T], F32, 2)
                    oBr = alloc(nc, bes, "oB", [64, 2, T], BF16, 2)

                    def loadB(hp):
                        ki, kTB, kwar = kTBr.next()
                        _, qTB, qwar = qTBr.next()
                        S.dma('sp', kTB[:], KT_scr[2 + hp], deps=kwar + qwar, stream=f"a{ki}")
                        for par_ in range(2):
                            tl = S.dma('sp', qTB[par_ * 64:(par_ + 1) * 64, par_, :], QT_scr[8 + hp, par_ * 64:(par_ + 1) * 64, :],
                                       deps=[('e', 'pool', S.cnt['pool'])], stream=f"a{ki}")
                        return ki, kTB, qTB, [tl]

                    nxt = loadB(0)
                    for hp in range(6):
                        ki, kTB, qTB, tl = nxt
                        if hp + 1 < 6:
                            nxt = loadB(hp + 1)
                        ai, accn, anwar = accnr.next()
                        _, accd, adwar = accdr.next()
                        lastpe = [None]
                        lastacc = [None, None]
                        for p, r in enumerate((1, 4, 16)):
                            span = 128 * r
                            nbh = T // span
                            vi, VB, vwar = VBr.next()
                            VBv = VB[:].rearrange("p (b r) c -> p b r c", r=r)
                            tv = []
                            for blk in range(nbh - 1, 2 * nbh):
                                srcv = V_scr[blk * span:(blk + 1) * span, 256 + hp * 128:256 + (hp + 1) * 128].rearrange(
                                    "(j r) c -> j r c", r=r)
                                tv.append(S.dma('sp', VBv[:, blk, :, :], srcv, deps=vwar, stream=f"vb{vi}"))
                            tv = [tv[-1]]
                            lastpv = [None]

                            def b_stage1(bt):
                                tiles = [((bt * 2 + u) // r, (bt * 2 + u) % r) for u in range(2)]
                                pts = []
                                for par in range(2):
                                    o = par * 64
                                    bi, bank, war = psA.next()
                                    S.op('pe', lambda e: e.matmul(bank[:], lhsT=identb[:],
                                                                  rhs=bB_b[:, p * 12 + hp * 2 + par].rearrange("k u w q -> k (u w q)"),
                                                                  start=True, stop=False), deps=tl + war, sig=False)
                                    tm = None
                                    n = 0
                                    for u, (blk, res) in enumerate(tiles):
                                        for w in range(2):
                                            kstart = (blk + nbh - 1 + w) * span + res
                                            qstart = blk * span + res
                                            tm = S.op('pe', lambda e: e.matmul(
                                                bank[:, (u * 2 + w) * 128:(u * 2 + w + 1) * 128],
                                                lhsT=kTB[:, sl(kstart, 128, r)],
                                                rhs=qTB[:, par, sl(qstart, 128, r)], start=False, stop=(n == 3)), sig=(n == 3))
                                            n += 1
                                    pi, pT, pwar = pTr.next()
                                    te = S.op('act', lambda e: e.activation(out=pT[:], in_=bank[:], func=AF.Exp), deps=[tm] + pwar)
                                    psA.done(bi, [te])
                                    pts.append((pi, pT, te))
                                return tiles, pts

                            def b_stage2(bt, stt_):
                                tiles, pts = stt_
                                nbi, nbank, nwar = psB.next()
                                dbi, dbank, dwar = psB.next()
                                tn = td_ = None
                                for par in range(2):
                                    pi, pT, te = pts[par]
                                    for u, (blk, res) in enumerate(tiles):
                                        for w in range(2):
                                            vt = (blk + nbh - 1 + w) * r + res
                                            tn = S.op('pe', lambda e: e.matmul(
                                                nbank[0:64, (par * 2 + u) * 128:(par * 2 + u + 1) * 128],
                                                lhsT=VB[:, vt, par * 64:(par + 1) * 64],
                                                rhs=pT[:, (u * 2 + w) * 128:(u * 2 + w + 1) * 128],
                                                start=(w == 0), stop=(w == 1)), deps=[te] + nwar + tv,
                                                sig=(par == 1 and u == 1 and w == 1))
                                for par in range(2):
                                    pi, pT, te = pts[par]
                                    for u, (blk, res) in enumerate(tiles):
                                        for w in range(2):
                                            lh = onesh_b[:, :] if (blk == 0 and w == 0) else ones_b[:, 0:64]
                                            td_ = S.op('pe', lambda e: e.matmul(
                                                dbank[0:64, (par * 2 + u) * 128:(par * 2 + u + 1) * 128],
                                                lhsT=lh, rhs=pT[:, (u * 2 + w) * 128:(u * 2 + w + 1) * 128],
                                                start=(w == 0), stop=(w == 1)), deps=[te] + dwar,
                                                sig=(par == 1 and u == 1 and w == 1))
                                for par in range(2):
                                    pTr.done(pts[par][0], [td_])

                                def dst(acc):
                                    (b0, r0), (b1, r1) = tiles
                                    if r == 1:
                                        return acc[:, :, b0 * 128:(b0 + 2) * 128].rearrange("p a (u j) -> p a u j", u=2)
                                    v_ = acc[:, :, b0 * span:(b0 + 1) * span].rearrange("p a (j r) -> p a r j", r=r)
                                    return v_[:, :, r0:r0 + 2, :]
                                srcn = nbank[0:64, :].rearrange("p (a u q) -> p a u q", a=2, u=2)
                                srcd = dbank[0:64, :].rearrange("p (a u q) -> p a u q", a=2, u=2)
                                if p == 0:
                                    a1 = S.op('act', lambda e: e.activation(out=dst(accn), in_=srcn, func=AF.Copy),
                                              deps=[tn] + anwar)
                                    a2 = S.op('act', lambda e: e.activation(out=dst(accd), in_=srcd, func=AF.Copy),
                                              deps=[td_] + adwar)
                                else:
                                    a1 = S.op('dve', lambda e: e.tensor_tensor(out=dst(accn), in0=srcn, in1=dst(accn), op=ALU.add),
                                              deps=[tn, lastacc[0], lastacc[1]])
                                    a2 = S.op('dve', lambda e: e.tensor_tensor(out=dst(accd), in0=srcd, in1=dst(accd), op=ALU.add),
                                              deps=[td_])
                                psB.done(nbi, [a1])
                                psB.done(dbi, [a2])
                                lastpv[0] = tn
                                lastpe[0] = td_
                                if p == 0:
                                    lastacc[0], lastacc[1] = a1, a2
                                else:
                                    lastacc[0], lastacc[1] = a1, a2

                            run_pipeline(8, b_stage1, b_stage2)
                            VBr.done(vi, [lastpv[0]])
                            bgc(3 if hp < 4 else 0)
                        kTBr.done(ki, [lastpe[0]])
                        qTBr.done(ki, [lastpe[0]])
                        oi, oB, owar = oBr.next()
                        f1 = S.op('dve', lambda e: e.reciprocal(out=accd[:], in_=accd[:]), deps=[lastacc[0], lastacc[1]])
                        f2 = S.op('pool', lambda e: e.tensor_tensor(out=oB[:], in0=accn[:], in1=accd[:], op=ALU.mult),
                                  deps=[f1, lastacc[0], lastacc[1]] + owar)
                        accnr.done(ai, [f2])
                        accdr.done(ai, [f2])
                        tds = []
                        for par in range(2):
                            tds.append(S.dma('pool', OT_scr[8 + hp, par * 64:(par + 1) * 64, :], oB[:, par, :], deps=[f2],
                                             stream=f"o{oi}"))
                        oBr.done(oi, [tds[-1]])
                    S.barrier()
            if phases <= 2.5:
                return

            with ExitStack() as pes:
                hT = alloc(nc, pes, "hT3", [128, KC, T], BF16)
                OT = alloc(nc, pes, "OT3", [128, 14, T], BF16)
                bg = alloc(nc, pes, "bg", [128, 32], F32)
                tl = [S.dma('sp', bg[:], b_gate, stream="c0")]
                for st in range(4):
                    tl.append(S.dma('sp', hT[:, :, st * 512:(st + 1) * 512], hT_scr[:, :, T + st * 512:T + (st + 1) * 512], stream="c0"))
                    tl.append(S.dma('sp', OT[:, :, st * 512:(st + 1) * 512],
                                    OT_scr[:, :, st * 512:(st + 1) * 512].rearrange("b p t -> p b t"), stream="c0"))
                tl = [tl[-1]]
                wr = alloc(nc, pes, "w3r", [128, 46, 128], BF16, 3)
                sgr = alloc(nc, pes, "sgr", [128, 512], F32, 4)
                zsr = alloc(nc, pes, "zsr", [128, 512], BF16, 3)
                wgv = w_gate.rearrange("(c p) n -> p c n", p=128)
                wav = w_a_out.rearrange("(c p) n -> p c n", p=128)
                wbv = w_b_out.rearrange("(c p) n -> p c n", p=128)
                def load3(fb_):
                    wi_, wt_, wwar_ = wr.next()
                    f0 = fb_ * 128
                    S.dma('pool', wt_[:, 0:16, :], wgv[:, :, f0:f0 + 128], deps=wwar_, stream=f"w{wi_}")
                    S.dma('pool', wt_[:, 16:32, :], wgv[:, :, D + f0:D + f0 + 128], stream=f"w{wi_}")
                    S.dma('pool', wt_[:, 32:40, :], wav[:, :, f0:f0 + 128], stream=f"w{wi_}")
                    tw_ = [S.dma('pool', wt_[:, 40:46, :], wbv[:, :, f0:f0 + 128], stream=f"w{wi_}")]
                    return wi_, wt_, tw_

                nxt3 = [load3(0), load3(1)]
                for fb in range(KC):
                    wi, wt, tw = nxt3.pop(0)
                    if fb + 2 < KC:
                        nxt3.append(load3(fb + 2))
                    lastpe = None
                    for st in range(4):
                        tsl = slice(st * 512, (st + 1) * 512)
                        banks = []
                        for (w0, nk, src, s0) in ((0, 16, hT, 0), (16, 16, hT, 0), (32, 8, OT, 0), (40, 6, OT, 8)):
                            bi, bank, war = psr.next()
                            tm = None
                            for c in range(nk):
                                tm = S.op('pe', lambda e: e.matmul(
                                    bank[:], lhsT=wt[:, w0 + c, :], rhs=src[:, s0 + c, tsl],
                                    start=(c == 0), stop=(c == nk - 1)),
                                    deps=tl + tw + war if c == 0 else (), sig=(c == nk - 1))
                            banks.append((bi, bank, tm))
                            lastpe = tm
                        ai, sga, awar = sgr.next()
                        bi2, sgb, bwar = sgr.next()
                        ta = S.op('act', lambda e: e.activation(out=sga[:], in_=banks[0][1][:], func=AF.Sigmoid,
                                                                bias=bg[:, fb:fb + 1], scale=1.0), deps=[banks[0][2]] + awar)
                        tb = S.op('act', lambda e: e.activation(out=sgb[:], in_=banks[1][1][:], func=AF.Sigmoid,
                                                                bias=bg[:, 16 + fb:17 + fb], scale=1.0), deps=[banks[1][2]] + bwar)
                        psr.done(banks[0][0], [ta])
                        psr.done(banks[1][0], [tb])
                        m1 = S.op('dve', lambda e: e.tensor_tensor(out=sga[:], in0=banks[2][1][:], in1=sga[:], op=ALU.mult),
                                  deps=[ta, banks[2][2]])
                        m2 = S.op('dve', lambda e: e.tensor_tensor(out=sgb[:], in0=banks[3][1][:], in1=sgb[:], op=ALU.mult),
                                  deps=[tb, banks[3][2]])
                        psr.done(banks[2][0], [m1])
                        psr.done(banks[3][0], [m2])
                        zi, zs, zwar = zsr.next()
                        m3 = S.op('pool', lambda e: e.tensor_tensor(out=zs[:], in0=sga[:], in1=sgb[:], op=ALU.add),
                                  deps=[m1, m2] + zwar)
                        sgr.done(ai, [m3])
                        sgr.done(bi2, [m3])
                        td = S.dma('sp', zT_scr[fb, :, tsl], zs[:], deps=[m3], stream=f"z{zi}")
                        zsr.done(zi, [td])
                    wr.done(wi, [lastpe])
                S.barrier()
            if phases <= 3:
                return

            with ExitStack() as pes:
                wo = alloc(nc, pes, "wo", [128, KC, D], BF16)
                wov = w_o.rearrange("(c p) n -> p c n", p=128)
                tw = []
                for c4 in range(4):
                    for hf in range(2):
                        tw.append(S.dma('pool', wo[:, c4 * 4:(c4 + 1) * 4, hf * 1024:(hf + 1) * 1024],
                                        wov[:, c4 * 4:(c4 + 1) * 4, hf * 1024:(hf + 1) * 1024], stream="w0"))
                tw = [tw[-1]]
                zr = alloc(nc, pes, "zr", [128, KC, 512], BF16, 2)
                xr = alloc(nc, pes, "x4r", [128, D], F32, 3)
                for st in range(4):
                    zi, zT, zwar = zr.next()
                    tz = S.dma('sp', zT[:], zT_scr[:, :, st * 512:(st + 1) * 512].rearrange("c p t -> p c t"), deps=zwar, stream=f"h{zi}")
                    lastpe = None
                    for tt in range(4):
                        r0 = st * 512 + tt * 128
                        xi, xt, xwar = xr.next()
                        tx = S.dma('sp', xt[:], xo[r0:r0 + 128, :], deps=xwar, stream=f"x{xi}")
                        tas = []
                        for cb in range(4):
                            bi, bank, war = psr.next()
                            tm = None
                            for c in range(KC):
                                tm = S.op('pe', lambda e: e.matmul(
                                    bank[:], lhsT=zT[:, c, tt * 128:(tt + 1) * 128], rhs=wo[:, c, cb * 512:(cb + 1) * 512],
                                    start=(c == 0), stop=(c == KC - 1)),
                                    deps=[tz] + tw + war if c == 0 else (), sig=(c == KC - 1))
                            ta = S.op('dve', lambda e: e.tensor_tensor(out=xt[:, cb * 512:(cb + 1) * 512], in0=bank[:],
                                                                       in1=xt[:, cb * 512:(cb + 1) * 512], op=ALU.add), deps=[tm, tx])
                            psr.done(bi, [ta])
                            tas.append(ta)
                            lastpe = tm
                        td = S.dma('act', x1_scr[r0:r0 + 128, :], xt[:], deps=tas, stream=f"x{xi}")
                        xr.done(xi, [td])
                    zr.done(zi, [lastpe])
                S.barrier()
            if phases <= 4:
                return

            slots_i = alloc(nc, es, "slots_i", [128, 32], I32)
            gates_f = alloc(nc, es, "gates_f", [128, 32], F32)
            with ExitStack() as pes:
                gx_bc = alloc(nc, pes, "gx_bc", [128, D], F32)
                gmoe_bc = alloc(nc, pes, "gmoe_bc", [128, D], F32)
                wxq = alloc(nc, pes, "wxq", [128, KC, 512], BF16)
                wxo = alloc(nc, pes, "wxo", [128, 4, D], BF16)
                wr_sb = alloc(nc, pes, "wr_sb", [128, KC, 36], F32)
                KxT = alloc(nc, pes, "KxT", [128, 4, 256], BF16)
                Vx = alloc(nc, pes, "Vx", [128, 2, 512], BF16)
                Umat = alloc(nc, pes, "Umat", [128, 128], BF16)
                eC = alloc(nc, pes, "eC", [128, 32], F32)
                eCi = alloc(nc, pes, "eCi", [128, 32], I32)
                TRf = alloc(nc, pes, "TRf", [128, 1], F32)
                TRi = alloc(nc, pes, "TRi", [128, 1], I32)
                cum = alloc(nc, pes, "cum", [128, 32], F32)
                junk = alloc(nc, pes, "junk4", [128, D], BF16)
                ssm = alloc(nc, pes, "ssm", [128, 8], F32)
                tset = []
                tset.append(S.dma('sp', gx_bc[:], g_x.partition_broadcast(128), stream="c0"))
                tset.append(S.dma('sp', gmoe_bc[:], g_moe.partition_broadcast(128), stream="c0"))
                tset.append(S.dma('sp', wr_sb[:], w_r.rearrange("(c p) n -> p c n", p=128), stream="c0"))
                tset.append(S.dma('pool', wxq[:], w_xq.rearrange("(c p) n -> p c n", p=128), stream="w0"))
                tset.append(S.dma('pool', wxo[:], w_xo.rearrange("(c p) n -> p c n", p=128), stream="w0"))
                tset.append(S.op('pool', lambda e: e.memset(Umat[:], 1.0)))
                tset.append(S.op('pool', lambda e: e.affine_select(out=Umat[:], in_=Umat[:], pattern=[[1, 128]], base=0,
                                                                   channel_multiplier=-1, compare_op=ALU.is_gt, fill=0.0)))
                tset.append(S.op('pool', lambda e: e.iota(eCi[:], pattern=[[CAP, 32]], base=0, channel_multiplier=0)))
                tset.append(S.op('pool', lambda e: e.tensor_copy(out=eC[:], in_=eCi[:]), ss=True))
                tset.append(S.op('pool', lambda e: e.iota(TRi[:], pattern=[[0, 1]], base=NEXP * CAP, channel_multiplier=1)))
                tset.append(S.op('pool', lambda e: e.tensor_copy(out=TRf[:], in_=TRi[:]), ss=True))
                tset.append(S.op('pool', lambda e: e.memset(cum[:], 0.0)))
                with ExitStack() as mes:
                    gm_bc = alloc(nc, mes, "gm_bc", [128, D], F32)
                    wxk = alloc(nc, mes, "wxk", [128, KC, 512], BF16)
                    wxv = alloc(nc, mes, "wxv", [128, KC, 512], BF16)
                    mt_ = alloc(nc, mes, "mt_", [128, D], F32, 2)
                    mb_ = alloc(nc, mes, "mb_", [128, D], BF16, 2)
                    mT = alloc(nc, mes, "mT", [128, KC, 256], BF16)
                    t1_ = S.dma('sp', gm_bc[:], g_mem.partition_broadcast(128), stream="c0")
                    t2_ = S.dma('pool', wxk[:], w_xk.rearrange("(c p) n -> p c n", p=128), stream="w1")
                    t3_ = S.dma('pool', wxv[:], w_xv.rearrange("(c p) n -> p c n", p=128), stream="w1")
                    tks = []
                    for m in range(2):
                        _, xt, _ = mt_.next()
                        _, hb, _ = mb_.next()
                        tl = S.dma('sp', xt[:], memb[m * 128:(m + 1) * 128, :], stream="c0")
                        t4, _t = rmsnorm_tile(xt[:], gm_bc[:], hb[:], ssm[:, m:m + 1], ssm[:, 2 + m:3 + m], ssm[:, 4 + m:5 + m],
                                              junk, [tl, t1_])
                        tks += transpose_tile(hb, mT[:, :, m * 128:(m + 1) * 128], [t4], [])
                    evs = []
                    for hd in range(4):
                        bi, bank, war = psr.next()
                        tm = None
                        for c in range(KC):
                            tm = S.op('pe', lambda e: e.matmul(bank[:, 0:256], lhsT=wxk[:, c, hd * 128:(hd + 1) * 128], rhs=mT[:, c, :],
                                                               start=(c == 0), stop=(c == KC - 1)),
                                      deps=tks + [t3_] + war if c == 0 else (), sig=(c == KC - 1))
                        te = S.op('act', lambda e: e.activation(out=KxT[:, hd, :], in_=bank[:, 0:256], func=AF.Copy), deps=[tm])
                        psr.done(bi, [te])
                        evs.append(te)
                    for m in range(2):
                        bi, bank, war = psr.next()
                        tm = None
                        for c in range(KC):
                            tm = S.op('pe', lambda e: e.matmul(bank[:], lhsT=mT[:, c, m * 128:(m + 1) * 128], rhs=wxv[:, c, :],
                                                               start=(c == 0), stop=(c == KC - 1)),
                                      deps=tks + [t3_] + war if c == 0 else (), sig=(c == KC - 1))
                        te = S.op('dve', lambda e: e.tensor_copy(out=Vx[:, m, :], in_=bank[:]), deps=[tm])
                        psr.done(bi, [te])
                        evs.append(te)
                    tset += evs
                    S.barrier()

                xr = alloc(nc, pes, "x5r", [128, D], F32, 2)
                hbr = alloc(nc, pes, "hb5", [128, D], BF16, 2)
                h2Tr = alloc(nc, pes, "h2T", [128, KC, 128], BF16, 2)
                qTr = alloc(nc, pes, "qT5", [128, 4, 128], BF16, 2)
                pTr = alloc(nc, pes, "pT5", [128, 8, 128], BF16, 2)
                rdr = alloc(nc, pes, "rd5", [128, 512], F32, 2)
                oxr = alloc(nc, pes, "ox5", [128, 4, 128], BF16, 2)
                h3fr = alloc(nc, pes, "h3f", [128, D], F32, 2)
                h3br = alloc(nc, pes, "h3b", [128, D], BF16, 2)
                h3Tr = alloc(nc, pes, "h3T", [128, KC, 128], F32, 2)
                ss5 = alloc(nc, pes, "ss5", [128, 96], F32)
                rt = alloc(nc, pes, "rt", [128, 512], F32)
                indb = alloc(nc, pes, "indb", [128, 32], BF16, 2)
                xscale = 128.0 ** -0.5
                cum_tok = []
                cum_tok_ = [[]]
                def p4_h1(t):
                        r0 = t * 128
                        xi, xt, xwar = xr.next()
                        tx = S.dma('sp', xt[:], x1_scr[r0:r0 + 128, :], deps=xwar, stream=f"x{xi}")
                        hi, hb, hwar = hbr.next()
                        t4, _t = rmsnorm_tile(xt[:], gx_bc[:], hb[:], ss5[:, t:t + 1], ss5[:, 16 + t:17 + t], ss5[:, 32 + t:33 + t],
                                              junk, [tx] + tset + hwar)
                        h2i, h2T, h2war = h2Tr.next()
                        tks = transpose_tile(hb, h2T[:, :, :], [t4], h2war)
                        hbr.done(hi, tks)
                        bi, bank, war = psr.next()
                        tm = None
                        for hd in range(4):
                            for c in range(KC):
                                tm = S.op('pe', lambda e: e.matmul(bank[:, hd * 128:(hd + 1) * 128], lhsT=wxq[:, c, hd * 128:(hd + 1) * 128],
                                                                   rhs=h2T[:, c, :], start=(c == 0), stop=(c == KC - 1)),
                                          deps=tks + war + tset if (c == 0 and hd == 0) else (), sig=(c == KC - 1 and hd == 3))
                        h2Tr.done(h2i, [tm])
                        qi, qT, qwar = qTr.next()
                        tq = S.op('act', lambda e: e.activation(out=qT[:].rearrange("p h t -> p (h t)"), in_=bank[:], func=AF.Copy, scale=xscale),
                                  deps=[tm] + qwar)
                        psr.done(bi, [tq])
                        pi, pT, pwar = pTr.next()
                        tes = []
                        tm2 = None
                        for hh2 in range(2):
                            bi, bank, war = psr.next()
                            for hl in range(2):
                                hd = hh2 * 2 + hl
                                for m in range(2):
                                    tm2 = S.op('pe', lambda e: e.matmul(bank[:, (hl * 2 + m) * 128:(hl * 2 + m + 1) * 128],
                                                                        lhsT=KxT[:, hd, m * 128:(m + 1) * 128], rhs=qT[:, hd, :],
                                                                        start=True, stop=True),
                                               deps=[tq] + war, sig=(hl == 1 and m == 1))
                            te = S.op('act', lambda e: e.activation(out=pT[:, hh2 * 4:(hh2 + 1) * 4, :].rearrange("p a t -> p (a t)"),
                                                                    in_=bank[:], func=AF.Exp), deps=[tm2] + pwar)
                            psr.done(bi, [te])
                            tes.append(te)
                        qTr.done(qi, [tm2])
                        nbi, nbank, nwar = psr.next()
                        dbi, dbank, dwar = psr.next()
                        tn = td_ = None
                        for hd in range(4):
                            for m in range(2):
                                tn = S.op('pe', lambda e: e.matmul(nbank[:, hd * 128:(hd + 1) * 128], lhsT=Vx[:, m, hd * 128:(hd + 1) * 128],
                                                                   rhs=pT[:, hd * 2 + m, :], start=(m == 0), stop=(m == 1)),
                                          deps=tes + nwar, sig=(hd == 3 and m == 1))
                        for hd in range(4):
                            for m in range(2):
                                td_ = S.op('pe', lambda e: e.matmul(dbank[:, hd * 128:(hd + 1) * 128], lhsT=ones_b[:],
                                                                    rhs=pT[:, hd * 2 + m, :], start=(m == 0), stop=(m == 1)),
                                           deps=tes + dwar, sig=(hd == 3 and m == 1))
                        pTr.done(pi, [td_])
                        ri, rd, rwar = rdr.next()
                        f1 = S.op('dve', lambda e: e.reciprocal(out=rd[:], in_=dbank[:]), deps=[td_] + rwar)
                        oi, ox, owar = oxr.next()
                        f2 = S.op('dve', lambda e: e.tensor_tensor(out=ox[:].rearrange("p h t -> p (h t)"), in0=nbank[:], in1=rd[:], op=ALU.mult),
                                  deps=[tn] + owar)
                        psr.done(dbi, [f1])
                        psr.done(nbi, [f2])
                        rdr.done(ri, [f2])
                        tas = []
                        tm3 = None
                        for cb in range(4):
                            bi, bank, war = psr.next()
                            for hd in range(4):
                                tm3 = S.op('pe', lambda e: e.matmul(bank[:], lhsT=ox[:, hd, :], rhs=wxo[:, hd, cb * 512:(cb + 1) * 512],
                                                                    start=(hd == 0), stop=(hd == 3)), deps=[f2] + war, sig=(hd == 3))
                            ta = S.op('dve', lambda e: e.tensor_tensor(out=xt[:, cb * 512:(cb + 1) * 512], in0=bank[:],
                                                                       in1=xt[:, cb * 512:(cb + 1) * 512], op=ALU.add), deps=[tm3, t4, _t])
                            psr.done(bi, [ta])
                            tas.append(ta)
                        oxr.done(oi, [tm3])
                        tdx = S.dma('sp', x2_scr[r0:r0 + 128, :], xt[:], deps=tas, stream=f"x{xi}")
                        return dict(xi=xi, xt=xt, tas=tas, tdx=tdx, r0=r0)

                def p4_h2(t, st_):
                        xi, xt, tas, tdx = st_["xi"], st_["xt"], st_["tas"], st_["tdx"]
                        cum_tok = cum_tok_[0]
                        fi, h3f, fwar = h3fr.next()
                        t5, _t5 = rmsnorm_tile(xt[:], gmoe_bc[:], h3f[:], ss5[:, 48 + t:49 + t], ss5[:, 64 + t:65 + t], ss5[:, 80 + t:81 + t],
                                               junk, tas + fwar)
                        xr.done(xi, [tdx, t5, _t5])
                        b3i, h3b, b3war = h3br.next()
                        tcb = S.op('act', lambda e: e.activation(out=h3b[:], in_=h3f[:], func=AF.Copy), deps=[t5] + b3war)
                        h3i, h3T, h3war = h3Tr.next()
                        tcs = []
                        tp = None
                        for q4 in range(4):
                            bi, bank, war = psr.next()
                            for j in range(4):
                                c = q4 * 4 + j
                                tp = S.op('pe', lambda e: e.transpose(out=bank[:, j * 128:(j + 1) * 128], in_=h3f[:, c * 128:(c + 1) * 128],
                                                                      identity=identf[:]), deps=[t5] + war, sig=(j == 3))
                            dst_ = h3T[:, q4 * 4:(q4 + 1) * 4, :].rearrange("p c t -> p (c t)")
                            if q4 % 2 == 0:
                                tc_ = S.op('act', lambda e: e.activation(out=dst_, in_=bank[:], func=AF.Copy), deps=[tp] + h3war)
                            else:
                                tc_ = S.op('dve', lambda e: e.tensor_copy(out=dst_, in_=bank[:]), deps=[tp] + h3war)
                            psr.done(bi, [tc_])
                            tcs.append(tc_)
                        h3fr.done(fi, [tp, tcb])
                        bi, bank, war = psr.next()
                        tml = None
                        for c in range(KC):
                            tml = S.op('pe', lambda e: e.matmul(bank[:, 0:36], lhsT=h3T[:, c, :], rhs=wr_sb[:, c, :],
                                                                start=(c == 0), stop=(c == KC - 1)), deps=tcs + war + tset, sig=(c == KC - 1))
                        h3Tr.done(h3i, [tml])
                        L = rt[:, 0:36]
                        gmax, ngmax, gsum, ggate = rt[:, 40:41], rt[:, 41:42], rt[:, 42:43], rt[:, 43:44]
                        goh, pen, j4 = rt[:, 44:48], rt[:, 48:52], rt[:, 52:56]
                        em, oh1, em2, oh2 = rt[:, 64:96], rt[:, 96:128], rt[:, 128:160], rt[:, 160:192]
                        m1, m2, d21, e21, w1_, w2_ = rt[:, 192:193], rt[:, 193:194], rt[:, 194:195], rt[:, 195:196], rt[:, 196:197], rt[:, 197:198]
                        posf, slotv, prod = rt[:, 224:256], rt[:, 256:288], rt[:, 288:320]
                        s1, p1, ov, tmp1 = rt[:, 320:321], rt[:, 321:322], rt[:, 322:323], rt[:, 323:324]
                        V_ = lambda fn, deps=(): S.op('dve', fn, deps=deps, ss=True)
                        tL = V_(lambda e: e.tensor_copy(out=L, in_=bank[:, 0:36]), deps=[tml] + cum_tok)
                        psr.done(bi, [tL])
                        V_(lambda e: e.tensor_reduce(out=gmax, in_=rt[:, 0:4], axis=AX.X, op=ALU.max))
                        V_(lambda e: e.tensor_scalar(out=ngmax, in0=gmax, scalar1=-1.0, scalar2=None, op0=ALU.mult))
                        tg1 = V_(lambda e: e.tensor_scalar(out=goh, in0=rt[:, 0:4], scalar1=gmax, scalar2=None, op0=ALU.is_equal))
                        tg2 = S.op('act', lambda e: e.activation(out=j4, in_=rt[:, 0:4], func=AF.Exp, bias=ngmax, scale=1.0, accum_out=gsum),
                                   deps=[tg1])
                        V_(lambda e: e.tensor_scalar(out=pen, in0=goh, scalar1=1.0, scalar2=1e30, op0=ALU.subtract, op1=ALU.mult))
                        V_(lambda e: e.tensor_tensor(out=em.rearrange("p (g x) -> p g x", g=4), in0=rt[:, 4:36].rearrange("p (g x) -> p g x", g=4),
                                                     in1=pen.unsqueeze(2).to_broadcast([128, 4, 8]), op=ALU.add))
                        V_(lambda e: e.tensor_reduce(out=m1, in_=em, axis=AX.X, op=ALU.max))
                        V_(lambda e: e.tensor_scalar(out=oh1, in0=em, scalar1=m1, scalar2=None, op0=ALU.is_equal))
                        V_(lambda e: e.scalar_tensor_tensor(out=em2, in0=oh1, scalar=-1e30, in1=em, op0=ALU.mult, op1=ALU.add))
                        V_(lambda e: e.tensor_reduce(out=m2, in_=em2, axis=AX.X, op=ALU.max))
                        V_(lambda e: e.tensor_scalar(out=oh2, in0=em2, scalar1=m2, scalar2=None, op0=ALU.is_equal))
                        td21 = V_(lambda e: e.tensor_tensor(out=d21, in0=m2, in1=m1, op=ALU.subtract))
                        te21 = S.op('act', lambda e: e.activation(out=e21, in_=d21, func=AF.Exp), deps=[td21])
                        ii, ind, iwar = indb.next()
                        tind = V_(lambda e: e.tensor_tensor(out=ind[:], in0=oh1, in1=oh2, op=ALU.add), deps=iwar)
                        bi, bank, war = psr.next()
                        S.op('pe', lambda e: e.matmul(bank[:, 0:32], lhsT=Umat[:], rhs=ind[:], start=True, stop=True), deps=[tind] + war + tset, sig=False)
                        tpos = S.op('pe', lambda e: e.matmul(bank[:, 32:64], lhsT=ones_b[:], rhs=ind[:], start=True, stop=True))
                        indb.done(ii, [tpos])
                        V_(lambda e: e.reciprocal(out=ggate, in_=gsum), deps=[tg2])
                        V_(lambda e: e.tensor_scalar(out=w2_, in0=e21, scalar1=1.0, scalar2=None, op0=ALU.add), deps=[te21])
                        V_(lambda e: e.reciprocal(out=w1_, in_=w2_))
                        V_(lambda e: e.tensor_tensor(out=w2_, in0=e21, in1=w1_, op=ALU.mult))
                        V_(lambda e: e.tensor_tensor(out=gates_f[:, t:t + 1], in0=w1_, in1=ggate, op=ALU.mult))
                        V_(lambda e: e.tensor_tensor(out=gates_f[:, 16 + t:17 + t], in0=w2_, in1=ggate, op=ALU.mult))
                        V_(lambda e: e.tensor_tensor(out=posf, in0=bank[:, 0:32], in1=cum[:], op=ALU.add), deps=[tpos])
                        tcum = V_(lambda e: e.tensor_tensor(out=cum[:], in0=bank[:, 32:64], in1=cum[:], op=ALU.add))
                        psr.done(bi, [tcum])
                        V_(lambda e: e.tensor_tensor(out=slotv, in0=posf, in1=eC[:], op=ALU.add))
                        tsl_ = []
                        for k_, oh in enumerate((oh1, oh2)):
                            V_(lambda e: e.scalar_tensor_tensor(out=prod, in0=oh, scalar=1.0, in1=slotv, op0=ALU.mult, op1=ALU.mult, accum_out=s1))
                            V_(lambda e: e.scalar_tensor_tensor(out=prod, in0=oh, scalar=1.0, in1=posf, op0=ALU.mult, op1=ALU.mult, accum_out=p1))
                            V_(lambda e: e.tensor_scalar(out=ov, in0=p1, scalar1=float(CAP), scalar2=None, op0=ALU.is_ge))
                            V_(lambda e: e.tensor_tensor(out=tmp1, in0=TRf[:], in1=s1, op=ALU.subtract))
                            V_(lambda e: e.scalar_tensor_tensor(out=tmp1, in0=tmp1, scalar=ov, in1=s1, op0=ALU.mult, op1=ALU.add))
                            tsl_.append(V_(lambda e: e.tensor_copy(out=slots_i[:, k_ * 16 + t:k_ * 16 + t + 1], in_=tmp1)))
                        cum_tok = [tsl_[-1]]
                        tsc = None
                        for k_ in range(2):
                            tsc = S.idma(out=X_disp, out_offset=bass.IndirectOffsetOnAxis(ap=slots_i[:, k_ * 16 + t:k_ * 16 + t + 1], axis=0),
                                         in_=h3b[:], in_offset=None, deps=[tsl_[k_], tcb], stream=f"sc{b3i}",
                                         bounds_check=NROWS - 1, oob_is_err=False)
                        h3br.done(b3i, [tsc])
                        bgc(3 if t < 6 else 0)
                        cum_tok_[0] = cum_tok

                p4s = {}
                for t in range(NT + 1):
                    if t < NT:
                        p4s[t] = p4_h1(t)
                    if t >= 1:
                        p4_h2(t - 1, p4s.pop(t - 1))
                bgc(1000)
                cum_tok = cum_tok_[0]
                if "route_scr" in taps:
                    slf = alloc(nc, pes, "slf", [128, 32], F32)
                    tq_ = S.op('dve', lambda e: e.tensor_copy(out=slf[:], in_=slots_i[:]), deps=cum_tok)
                    S.dma('sp', route_scr[:, 0:2, :], gates_f[:].rearrange("p (k t) -> p k t", k=2), deps=[tq_], stream="c0")
                    S.dma('sp', route_scr[:, 2:4, :], slf[:].rearrange("p (k t) -> p k t", k=2), deps=[tq_], stream="c0")
                S.barrier(skip=())
            if phases <= 5:
                return

            with ExitStack() as pes:
                w13r = alloc(nc, pes, "w13", [128, 2, KC, 512], BF16, 2)
                Xr = alloc(nc, pes, "Xr", [128, D], BF16, 4)
                XTr = alloc(nc, pes, "XT", [128, KC, 256], BF16, 2)
                sr = alloc(nc, pes, "s6", [128, 256], F32, 3)
                aTr = alloc(nc, pes, "aT", [128, 4, 256], BF16, 2)
                yr = alloc(nc, pes, "y6", [128, D], F32, 3)
                zt = alloc(nc, pes, "zt6", [128, D], F32)
                tz = S.op('pool', lambda e: e.memset(zt[:], 0.0))
                S.dma('sp', Y_disp[NEXP * CAP:NEXP * CAP + 128, :], zt[:], deps=[tz], stream="c0")
                flip = 0
                w2r3 = alloc(nc, pes, "w2c", [128, 4, D], BF16, 3)
                flip_ = [0]

                xl = {}

                def loadX(ex_):
                    lst = []
                    for rb in range(2):
                        xi, Xt, xwar = Xr.next()
                        r0 = ex_ * CAP + rb * 128
                        tx = S.dma('pool', Xt[:], X_disp[r0:r0 + 128, :], deps=xwar, stream=f"X{xi}")
                        lst.append((xi, Xt, tx))
                    xl[ex_] = lst

                loadX(0)

                def stA(ex):
                    if ex + 1 < NEXP:
                        loadX(ex + 1)
                    wi_, w13_, wwar_ = w13r.next()
                    w2i_, w2b_, w2war_ = w2r3.next()
                    S.dma('pool', w13_[:, 0], w1b_scr[ex], deps=wwar_, stream=f"w{wi_}")
                    tw13 = S.dma('pool', w13_[:, 1], w3b_scr[ex], stream=f"w{wi_}")
                    tw2 = S.dma('pool', w2b_[:], w2b_scr[ex], deps=w2war_, stream=f"u{w2i_}")
                    xti, XT, xtwar = XTr.next()
                    tcs = []
                    for rb, (xi, Xt, tx) in enumerate(xl.pop(ex)):
                        tks = transpose_tile(Xt, XT[:, :, rb * 128:(rb + 1) * 128], [tx], xtwar)
                        Xr.done(xi, tks)
                        tcs += tks
                    return dict(wi=wi_, w13=w13_, tw13=[tw13], w2i=w2i_, w2b=w2b_, tw2=[tw2], xti=xti, XT=XT, tcs=tcs)

                def stB(ex, st):
                    w13, XT = st["w13"], st["XT"]
                    ai, aT, awar = aTr.next()
                    tm = None
                    tmul = []
                    for fo in range(4):
                        bi, bank, war = psr.next()
                        for half in range(2):
                            for c in range(KC):
                                tm = S.op('pe', lambda e: e.matmul(
                                    bank[:, half * 256:(half + 1) * 256], lhsT=w13[:, half, c, fo * 128:(fo + 1) * 128],
                                    rhs=XT[:, c, :], start=(c == 0), stop=(c == KC - 1)),
                                    deps=st["tcs"] + st["tw13"] + war if (c == 0 and half == 0) else (), sig=(c == KC - 1 and half == 1))
                        si, s_, swar = sr.next()
                        ts = S.op('act', lambda e: e.activation(out=s_[:], in_=bank[:, 0:256], func=AF.Silu), deps=[tm] + swar)
                        tmu = S.op('dve', lambda e: e.tensor_tensor(out=aT[:, fo, :], in0=bank[:, 256:512], in1=s_[:], op=ALU.mult),
                                   deps=[ts, tm] + awar)
                        psr.done(bi, [tmu])
                        sr.done(si, [tmu])
                        tmul.append(tmu)
                    XTr.done(st["xti"], [tm])
                    w13r.done(st["wi"], [tm])
                    st.update(ai=ai, aT=aT, tmul=tmul)

                def stC(ex, st):
                    aT, w2b = st["aT"], st["w2b"]
                    tm2 = None
                    for rb in range(2):
                        yi, yt, ywar = yr.next()
                        tes = []
                        for cb in range(4):
                            bi, bank, war = psr.next()
                            for fo in range(4):
                                tm2 = S.op('pe', lambda e: e.matmul(bank[:], lhsT=aT[:, fo, rb * 128:(rb + 1) * 128],
                                                                    rhs=w2b[:, fo, cb * 512:(cb + 1) * 512], start=(fo == 0), stop=(fo == 3)),
                                           deps=st["tmul"] + st["tw2"] + war if fo == 0 else (), sig=(fo == 3))
                            flip_[0] ^= 1
                            if flip_[0]:
                                te = S.op('act', lambda e: e.activation(out=yt[:, cb * 512:(cb + 1) * 512], in_=bank[:], func=AF.Copy),
                                          deps=[tm2] + ywar)
                            else:
                                te = S.op('dve', lambda e: e.tensor_copy(out=yt[:, cb * 512:(cb + 1) * 512], in_=bank[:]), deps=[tm2] + ywar)
                            psr.done(bi, [te])
                            tes.append(te)
                        r0 = ex * CAP + rb * 128
                        td = S.dma('sp', Y_disp[r0:r0 + 128, :], yt[:], deps=tes, stream=f"y{yi}")
                        yr.done(yi, [td])
                    aTr.done(st["ai"], [tm2])
                    w2r3.done(st["w2i"], [tm2])

                sts = {}
                for step in range(NEXP + 2):
                    if step < NEXP:
                        sts[step] = stA(step)
                    if 0 <= step - 1 < NEXP:
                        stB(step - 1, sts[step - 1])
                    if 0 <= step - 2 < NEXP:
                        stC(step - 2, sts.pop(step - 2))
                S.barrier()
            if phases <= 6:
                return

            with ExitStack() as pes:
                gf_bc = alloc(nc, pes, "gf_bc", [128, D], F32)
                tg = S.dma('sp', gf_bc[:], g_final.partition_broadcast(128), stream="c0")
                xr = alloc(nc, pes, "x7r", [128, D], F32, 2)
                y1r = alloc(nc, pes, "y1r", [128, D], F32, 2)
                y2r = alloc(nc, pes, "y2r", [128, D], F32, 2)
                outr = alloc(nc, pes, "outr", [128, D], F32, 2)
                junk = alloc(nc, pes, "junk7", [128, D], BF16)
                ss7 = alloc(nc, pes, "ss7", [128, 48], F32)
                tout = []
                for t in range(NT):
                    r0 = t * 128
                    xi, xt, xwar = xr.next()
                    tx = S.dma('sp', xt[:], x2_scr[r0:r0 + 128, :], deps=xwar, stream=f"x{xi}")
                    y1i, y1, y1war = y1r.next()
                    y2i, y2, y2war = y2r.next()
                    tg1 = S.idma(out=y1[:], out_offset=None, in_=Y_disp, in_offset=bass.IndirectOffsetOnAxis(ap=slots_i[:, t:t + 1], axis=0),
                                 deps=y1war, stream=f"g1{y1i}", bounds_check=NROWS - 1, oob_is_err=False)
                    tg2 = S.idma(out=y2[:], out_offset=None, in_=Y_disp, in_offset=bass.IndirectOffsetOnAxis(ap=slots_i[:, 16 + t:17 + t], axis=0),
                                 deps=y2war, stream=f"g2{y2i}", bounds_check=NROWS - 1, oob_is_err=False)
                    c1 = S.op('dve', lambda e: e.scalar_tensor_tensor(out=xt[:], in0=y1[:], scalar=gates_f[:, t:t + 1], in1=xt[:],
                                                                      op0=ALU.mult, op1=ALU.add), deps=[tx, tg1])
                    c2 = S.op('dve', lambda e: e.scalar_tensor_tensor(out=xt[:], in0=y2[:], scalar=gates_f[:, 16 + t:17 + t], in1=xt[:],
                                                                      op0=ALU.mult, op1=ALU.add), deps=[tg2])
                    y1r.done(y1i, [c1])
                    y2r.done(y2i, [c2])
                    oi, ot, owar = outr.next()
                    t4, t1 = rmsnorm_tile(xt[:], gf_bc[:], ot[:], ss7[:, t:t + 1], ss7[:, 16 + t:17 + t], ss7[:, 32 + t:33 + t], junk,
                                          [c2, tg] + owar)
                    xr.done(xi, [t4, t1])
                    td = S.dma('sp', out[r0:r0 + 128, :], ot[:], deps=[t4], stream=f"o{oi}")
                    outr.done(oi, [td])
                    tout.append(td)
                S.barrier()

    return nc


def _t5_bucket(d):
    d = np.maximum(d, 0)
    df = np.maximum(d, 1).astype(np.float32)
    large = 16 + (np.log(df / np.float32(16)) / np.float32(math.log(2048 / 16)) * np.float32(16)).astype(np.int32)
    large = np.minimum(large, 31)
    return np.where(d < 16, d, large)


def _bias_tables(rel_bias):
    k = np.arange(128)[:, None]
    q = np.arange(128)[None, :]
    dist = np.stack([q + 128 - k, q - k], axis=0)
    bA = rel_bias[_t5_bucket(dist)][..., :16]
    biasA = np.transpose(bA, (1, 3, 0, 2)).reshape(128, 4, 2, 2, 2, 128)
    biasA = np.ascontiguousarray(np.transpose(biasA, (0, 1, 3, 2, 4, 5))).astype(np.float32)
    bBs = []
    for (_w, r) in ((128, 1), (512, 4), (2048, 16)):
        bb = rel_bias[_t5_bucket(dist * r)][..., 16:]
        bBs.append(np.transpose(bb, (1, 3, 0, 2)))
    biasB = np.ascontiguousarray(np.stack(bBs, axis=1)).astype(np.float32)
    okA = (dist >= 0) & (dist <= 127)
    okB = (dist >= 0) & (dist <= 128)
    maskA = np.ascontiguousarray(np.transpose(np.where(okA, 0.0, NEGM), (1, 0, 2))).astype(np.float32)
    maskB = np.ascontiguousarray(np.transpose(np.where(okB, 0.0, NEGM), (1, 0, 2))).astype(np.float32)
    return biasA, biasB, maskA, maskB


def make_in_maps(inputs, phases=99, cores=range(NCORES)):
    x = np.asarray(inputs["x"], dtype=np.float32)
    rel_bias = np.asarray(inputs["rel_bias"], dtype=np.float32)
    biasA, biasB, maskA, maskB = _bias_tables(rel_bias)
    sq = lambda n: np.ascontiguousarray(np.asarray(inputs[n], dtype=np.float32)[0])
    common = dict(g_mix=sq("g_mix")[None, :], w_in=sq("w_in"), biasA=biasA, biasB=biasB, maskA=maskA, maskB=maskB,
                  sinks=np.ascontiguousarray(sq("sinks_a").reshape(4, 2, 2).transpose(0, 2, 1).reshape(1, 16)))
    if phases >= 3:
        common.update(w_a_out=sq("w_a_out"), w_b_out=sq("w_b_out"), w_gate=sq("w_gate"),
                      b_gate=np.ascontiguousarray(sq("b_gate").reshape(32, 128).T))
    if phases >= 4:
        common.update(w_o=sq("w_o"))
    if phases >= 5:
        common.update(g_x=sq("g_x")[None, :], g_mem=sq("g_mem")[None, :], g_moe=sq("g_moe")[None, :],
                      w_xq=sq("w_xq"), w_xk=sq("w_xk"), w_xv=sq("w_xv"), w_xo=sq("w_xo"),
                      w_r=np.ascontiguousarray(np.concatenate([sq("w_rg"), sq("w_re")], axis=1)))
    if phases >= 6:
        common.update(w1=sq("w1"), w3=sq("w3"), w2=sq("w2"))
    if phases >= 7:
        common.update(g_final=np.asarray(inputs["g_final"], dtype=np.float32)[None, :])
    maps = []
    for c in cores:
        b, qd = c // 4, c % 4
        m = dict(common)
        m["xo"] = np.ascontiguousarray(x[b, qd * T:(qd + 1) * T])
        if qd > 0:
            m["xh"] = np.ascontiguousarray(x[b, (qd - 1) * T:qd * T])
            m["hvalid"] = np.ones((128, 64), np.float32)
        else:
            m["xh"] = np.zeros((T, D), np.float32)
            m["hvalid"] = np.zeros((128, 64), np.float32)
        if phases >= 5:
            m["memb"] = np.ascontiguousarray(np.asarray(inputs["mem"], dtype=np.float32)[b])
        maps.append(m)
    return maps


def kernel(**inputs):
    nc = build()
    maps = make_in_maps(inputs)
    res = run_bass_kernel_spmd(nc, maps, core_ids=list(range(NCORES)))
    outs = [r["out"] for r in res.results]
    full = np.stack([np.concatenate(outs[b * 4:(b + 1) * 4], axis=0) for b in range(2)], axis=0)
    return full.astype(np.float32)
```

```python
import math
from contextlib import ExitStack

import numpy as np
import concourse.bass as bass
import concourse.mybir as mybir
from concourse.bass_utils import run_bass_kernel_spmd

F32 = mybir.dt.float32
BF16 = mybir.dt.bfloat16
I32 = mybir.dt.int32
AF = mybir.ActivationFunctionType
ALU = mybir.AluOpType
AX = mybir.AxisListType

D = 2048
T = 2048
NT = 16
NST = 4
KC = 16
NCORES = 8
CAP = 256
NEXP = 32
NROWS = NEXP * CAP + 128
EPS = 1e-6
NEGM = -30000.0


class Sched:
    def __init__(self, nc, es):
        self.nc = nc
        self.es = es
        self.eng = dict(pe=nc.tensor, act=nc.scalar, dve=nc.vector, pool=nc.gpsimd, sp=nc.sync)
        self.sem = {k: es.enter_context(nc.semaphore("s_" + k)) for k in self.eng}
        self.cnt = {k: 0 for k in self.eng}
        self.seen = {k: {} for k in self.eng}
        self.dsem = {}

    def wait(self, e, deps):
        for d in deps:
            if d is None:
                continue
            if isinstance(d, list):
                self.wait(e, d)
                continue
            kind, key, val = d
            if kind == 'e' and key == e:
                continue
            sk = (kind, key)
            if self.seen[e].get(sk, 0) >= val:
                continue
            sem = self.sem[key] if kind == 'e' else self.dsem[key][0]
            self.eng[e].wait_ge(sem, val)
            self.seen[e][sk] = val

    def op(self, e, fn, deps=(), sig=True, ss=False):
        self.wait(e, deps)
        if ss and self.cnt[e] > 0 and self.seen[e].get(('e', e), 0) < self.cnt[e]:
            self.eng[e].wait_ge(self.sem[e], self.cnt[e])
            self.seen[e][('e', e)] = self.cnt[e]
        ins = fn(self.eng[e])
        if sig:
            self.cnt[e] += 1
            ins.then_inc(self.sem[e], 1)
            return ('e', e, self.cnt[e])
        return ('e', e, self.cnt[e] + 1)

    def dma(self, q, out, in_, deps=(), stream="d", **kw):
        self.wait(q, deps)
        for d in deps:
            if isinstance(d, tuple) and d[0] == 'e' and d[1] == q and self.seen[q].get(('e', q), 0) < d[2]:
                self.eng[q].wait_ge(self.sem[q], d[2])
                self.seen[q][('e', q)] = d[2]
        if stream not in self.dsem:
            self.dsem[stream] = [self.es.enter_context(self.nc.semaphore("d_" + stream)), 0]
        s = self.dsem[stream]
        s[1] += 16
        self.eng[q].dma_start(out=out, in_=in_, **kw).then_inc(s[0], 16)
        return ('d', stream, s[1])

    def idma(self, out, out_offset, in_, in_offset, deps=(), stream="i", **kw):
        q = 'pool'
        self.wait(q, deps)
        if isinstance(kw.get("bounds_check"), int):
            if not hasattr(self, "_bc"):
                self._bc = {}
            v = kw["bounds_check"]
            if v not in self._bc:
                self._bc[v] = self.eng[q].to_reg(v)
            kw["bounds_check"] = self._bc[v]
        if stream not in self.dsem:
            self.dsem[stream] = [self.es.enter_context(self.nc.semaphore("d_" + stream)), 0]
        s = self.dsem[stream]
        s[1] += 16
        self.eng[q].indirect_dma_start(out=out, out_offset=out_offset, in_=in_, in_offset=in_offset,
                                       **kw).then_inc(s[0], 16)
        return ('d', stream, s[1])

    def all_tokens(self, skip=()):
        toks = [('e', k, v) for k, v in self.cnt.items() if v > 0]
        toks += [('d', k, v[1]) for k, v in self.dsem.items() if v[1] > 0 and k not in skip]
        return toks

    def barrier(self, skip=("bg",)):
        toks = self.all_tokens(skip)
        for e in self.eng:
            self.wait(e, toks)


class Ring:
    def __init__(self, tiles):
        self.tiles = tiles
        self.war = [[] for _ in tiles]
        self.i = -1

    def next(self):
        self.i = (self.i + 1) % len(self.tiles)
        return self.i, self.tiles[self.i], self.war[self.i]

    def done(self, i, toks):
        self.war[i] = [t for t in toks if t is not None]


def alloc(nc, es, name, shape, dt, n=None):
    if n is None:
        return es.enter_context(nc.sbuf_tensor(name, shape, dt))
    return Ring([es.enter_context(nc.sbuf_tensor(f"{name}{i}", shape, dt)) for i in range(n)])


def build(phases=99, taps=()):
    nc = bass.Bass("TRN2", target_bir_lowering=False)

    def din(name, shape, dt=F32):
        return nc.dram_tensor(name, list(shape), dt, kind="ExternalInput").ap()

    def dscr(name, shape, dt, tap=False):
        kind = "ExternalOutput" if name in taps else "Internal"
        return nc.dram_tensor(name, list(shape), dt, kind=kind).ap()

    xo = din("xo", [T, D])
    xh = din("xh", [T, D])
    hvalid = din("hvalid", [128, 64])
    g_mix = din("g_mix", [1, D])
    w_in = din("w_in", [D, 3840])
    biasA = din("biasA", [128, 4, 2, 2, 2, 128])
    biasB = din("biasB", [128, 3, 12, 2, 128])
    maskA = din("maskA", [128, 2, 128])
    maskB = din("maskB", [128, 2, 128])
    sinks = din("sinks", [1, 16])
    if phases >= 3:
        w_a_out = din("w_a_out", [1024, D])
        w_b_out = din("w_b_out", [768, D])
        w_gate = din("w_gate", [D, 2 * D])
        b_gate = din("b_gate", [128, 32])
    if phases >= 4:
        w_o = din("w_o", [D, D])
    if phases >= 5:
        memb = din("memb", [256, D])
        g_x = din("g_x", [1, D])
        g_mem = din("g_mem", [1, D])
        g_moe = din("g_moe", [1, D])
        w_xq = din("w_xq", [D, 512])
        w_xk = din("w_xk", [D, 512])
        w_xv = din("w_xv", [D, 512])
        w_xo = din("w_xo", [512, D])
        w_r = din("w_r", [D, 36])
    if phases >= 6:
        w1 = din("w1", [NEXP, D, 512])
        w3 = din("w3", [NEXP, D, 512])
        w2 = din("w2", [NEXP, 512, D])
    if phases >= 7:
        g_final = din("g_final", [1, D])
        out = nc.dram_tensor("out", [T, D], F32, kind="ExternalOutput").ap()

    hT_scr = dscr("hT_scr", [128, KC, 2 * T], BF16)
    QT_scr = dscr("QT_scr", [14, 128, T], BF16)
    KT_scr = dscr("KT_scr", [8, 128, 2 * T], BF16)
    V_scr = dscr("V_scr", [2 * T, 1024], BF16)
    OT_scr = dscr("OT_scr", [14, 128, T], BF16)
    zT_scr = dscr("zT_scr", [KC, 128, T], BF16)
    x1_scr = dscr("x1_scr", [T, D], F32)
    x2_scr = dscr("x2_scr", [T, D], F32)
    X_disp = dscr("X_disp", [NROWS, D], BF16)
    Y_disp = dscr("Y_disp", [NROWS, D], F32)
    route_scr = dscr("route_scr", [128, 4, NT], F32)
    if phases >= 6:
        w1b_scr = dscr("w1b_scr", [NEXP, 128, KC, 512], BF16)
        w3b_scr = dscr("w3b_scr", [NEXP, 128, KC, 512], BF16)
        w2b_scr = dscr("w2b_scr", [NEXP, 128, 4, D], BF16)

    with ExitStack() as es:
        S = Sched(nc, es)
        ps = [es.enter_context(nc.psum_tensor(f"ps{i}", [128, 512], F32)) for i in range(8)]
        psr = Ring(ps)
        block = es.enter_context(nc.Block())

        @block.gpsimd
        def _(_g):
            identb = alloc(nc, es, "identb", [128, 128], BF16)
            identf = alloc(nc, es, "identf", [128, 128], F32)
            ones_b = alloc(nc, es, "ones_b", [128, 128], BF16)
            onesh_b = alloc(nc, es, "onesh_b", [128, 64], BF16)
            hv_f = alloc(nc, es, "hv_f", [128, 64], F32)
            eps_t = alloc(nc, es, "eps_t", [128, 1], F32)
            t_c = []
            t_c.append(S.op('pool', lambda e: e.memset(identf[:], 0.0)))
            t_c.append(S.op('pool', lambda e: e.affine_select(
                out=identf[:], in_=identf[:], pattern=[[-1, 128]], base=0, channel_multiplier=1,
                compare_op=ALU.not_equal, fill=1.0)))
            t_c.append(S.op('pool', lambda e: e.tensor_copy(out=identb[:], in_=identf[:])))
            t_c.append(S.op('pool', lambda e: e.memset(ones_b[:], 1.0)))
            t_c.append(S.op('pool', lambda e: e.memset(eps_t[:], EPS)))
            t = S.dma('sp', hv_f[:], hvalid, stream="c0")
            t_c.append(S.op('dve', lambda e: e.tensor_copy(out=onesh_b[:], in_=hv_f[:]), deps=[t]))
            C = dict(identb=identb, identf=identf, ones_b=ones_b, onesh_b=onesh_b, eps_t=eps_t)
            S.barrier()

            bg_list = []
            if phases >= 6:
                for ex_ in range(NEXP):
                    bg_list += [(w1b_scr[ex_], w1[ex_].rearrange("(c p) n -> p c n", p=128)),
                                (w3b_scr[ex_], w3[ex_].rearrange("(c p) n -> p c n", p=128)),
                                (w2b_scr[ex_], w2[ex_].rearrange("(c p) n -> p c n", p=128))]
            bg_state = [0]

            def bgc(n=1):
                for _ in range(n):
                    if bg_state[0] < len(bg_list):
                        o_, i_ = bg_list[bg_state[0]]
                        bg_state[0] += 1
                        S.dma('pool', o_, i_, stream="bg")

            def rmsnorm_tile(xt, g_bc, hb_out, ss_col, tmp_col, rstd_col, junk, deps):
                t1 = S.op('act', lambda e: e.activation(out=junk[:], in_=xt, func=AF.Square,
                                                        accum_out=ss_col), deps=deps)
                t2 = S.op('act', lambda e: e.activation(out=tmp_col, in_=ss_col, func=AF.Sqrt,
                                                        bias=eps_t[:], scale=1.0 / D), deps=[t1], ss=True)
                t3 = S.op('dve', lambda e: e.reciprocal(out=rstd_col, in_=tmp_col), deps=[t2])
                t4 = S.op('dve', lambda e: e.scalar_tensor_tensor(
                    out=hb_out, in0=xt, scalar=rstd_col, in1=g_bc, op0=ALU.mult, op1=ALU.mult),
                    deps=[t3] + list(deps), ss=True)
                return t4, t1

            def transpose_tile(hb, dstT, deps, war_dst):
                toks = []
                for half in range(2):
                    bi, bank, war = psr.next()
                    pv = bank[:].bitcast(BF16)
                    tp = None
                    for j in range(8):
                        c = half * 8 + j
                        tp = S.op('pe', lambda e, c=c, j=j: e.transpose(
                            out=pv[:, j * 128:(j + 1) * 128], in_=hb[:, c * 128:(c + 1) * 128],
                            identity=identb[:]), deps=list(deps) + war if j == 0 else (), sig=(j == 7))
                    eng = 'act' if half == 0 else 'dve'
                    src = pv.rearrange("p (c t) -> p c t", t=128)
                    dst = dstT[:, half * 8:(half + 1) * 8, :]
                    if eng == 'act':
                        tc_ = S.op('act', lambda e: e.activation(out=dst, in_=src, func=AF.Copy),
                                   deps=[tp] + war_dst)
                    else:
                        tc_ = S.op('dve', lambda e: e.tensor_copy(out=dst, in_=src), deps=[tp] + war_dst)
                    psr.done(bi, [tc_])
                    toks.append(tc_)
                return toks

            with ExitStack() as pes:
                g_bc = alloc(nc, pes, "g_bc", [128, D], F32)
                tg = S.dma('sp', g_bc[:], g_mix.partition_broadcast(128), stream="c0")
                xr = alloc(nc, pes, "xr", [128, D], F32, 3)
                hbr = alloc(nc, pes, "hbr", [128, D], BF16, 3)
                junk = alloc(nc, pes, "junk", [128, D], BF16)
                hTr = alloc(nc, pes, "hTr", [128, KC, 512], BF16, 2)
                ss = alloc(nc, pes, "ss", [128, 32], F32)
                tmpc = alloc(nc, pes, "tmpc", [128, 32], F32)
                rstd = alloc(nc, pes, "rstd", [128, 32], F32)
                def p1_s1(t):
                    src_ = xh if t < 16 else xo
                    r0 = (t % 16) * 128
                    xi, xt, xwar = xr.next()
                    tl = S.dma('sp', xt[:], src_[r0:r0 + 128, :], deps=xwar, stream=f"x{xi}")
                    bi, hb, bwar = hbr.next()
                    t4, t1 = rmsnorm_tile(xt[:], g_bc[:], hb[:], ss[:, t:t + 1], tmpc[:, t:t + 1],
                                          rstd[:, t:t + 1], junk, [tl, tg] + bwar)
                    xr.done(xi, [t4, t1])
                    return bi, hb, t4

                cur = {}

                def p1_s2(t, st_):
                    bi, hb, t4 = st_
                    st, tt = t // 4, t % 4
                    if tt == 0:
                        hi, hT, hwar = hTr.next()
                        cur.update(hi=hi, hT=hT, hwar=hwar, tcs=[])
                    tks = transpose_tile(hb, cur["hT"][:, :, tt * 128:(tt + 1) * 128], [t4], cur["hwar"])
                    hbr.done(bi, tks)
                    cur["tcs"] += tks
                    if tt == 3:
                        td = S.dma('act', hT_scr[:, :, st * 512:(st + 1) * 512], cur["hT"][:], deps=cur["tcs"], stream=f"h{cur['hi']}")
                        hTr.done(cur["hi"], [td])

                run_pipeline2 = None
                stt_ = {}
                for t in range(33):
                    if t < 32:
                        stt_[t] = p1_s1(t)
                    if t >= 1:
                        p1_s2(t - 1, stt_.pop(t - 1))
                S.barrier()
            if phases <= 1:
                return

            with ExitStack() as pes:
                wq = alloc(nc, pes, "wq", [128, KC, 1792], BF16)
                wkv = alloc(nc, pes, "wkv", [128, KC, 2048], BF16)
                w_in_v = w_in.rearrange("(c p) n -> p c n", p=128)
                tw = []
                for c4 in range(4):
                    tw.append(S.dma('pool', wq[:, c4 * 4:(c4 + 1) * 4, 0:1024], w_in_v[:, c4 * 4:(c4 + 1) * 4, 0:1024],
                                    stream="w0"))
                    tw.append(S.dma('pool', wq[:, c4 * 4:(c4 + 1) * 4, 1024:1792],
                                    w_in_v[:, c4 * 4:(c4 + 1) * 4, 1536:2304], stream="w0"))
                tw = [tw[-1]]
                twkv = []
                for c4 in range(4):
                    twkv.append(S.dma('pool', wkv[:, c4 * 4:(c4 + 1) * 4, 0:512], w_in_v[:, c4 * 4:(c4 + 1) * 4, 1024:1536],
                                      stream="w1"))
                    twkv.append(S.dma('pool', wkv[:, c4 * 4:(c4 + 1) * 4, 512:2048],
                                      w_in_v[:, c4 * 4:(c4 + 1) * 4, 2304:3840], stream="w1"))
                twkv = [twkv[-1]]
                hTr = alloc(nc, pes, "hTq", [128, KC, 512], BF16, 2)
                qsr = alloc(nc, pes, "qsr", [128, 512], BF16, 4)
                for st in range(4):
                    hi, hT, hwar = hTr.next()
                    tl = S.dma('sp', hT[:], hT_scr[:, :, T + st * 512:T + (st + 1) * 512], deps=hwar, stream=f"h{hi}")
                    last = []
                    for fb in range(14):
                        bi, bank, war = psr.next()
                        tm = None
                        for c in range(KC):
                            tm = S.op('pe', lambda e, c=c: e.matmul(
                                bank[:], lhsT=wq[:, c, fb * 128:(fb + 1) * 128], rhs=hT[:, c, :],
                                start=(c == 0), stop=(c == KC - 1)),
                                deps=[tl] + tw + war if c == 0 else (), sig=(c == KC - 1))
                        qi, qs, qwar = qsr.next()
                        te = S.op('act', lambda e: e.activation(out=qs[:], in_=bank[:], func=AF.Copy, scale=0.125),
                                  deps=[tm] + qwar)
                        psr.done(bi, [te])
                        td = S.dma('act', QT_scr[fb, :, st * 512:(st + 1) * 512], qs[:], deps=[te], stream=f"q{qi}")
                        qsr.done(qi, [td])
                        last = [tm]
                    hTr.done(hi, last)
                    bgc(3)
                tw = twkv
                ksr = alloc(nc, pes, "ksr", [128, 512], BF16, 4)
                vsr = alloc(nc, pes, "vsr", [128, 1024], BF16, 2)
                flip = 0
                for st in range(8):
                    hi, hT, hwar = hTr.next()
                    tl = S.dma('sp', hT[:], hT_scr[:, :, st * 512:(st + 1) * 512], deps=hwar, stream=f"h{hi}")
                    last = []
                    for kb in range(8):
                        if kb < 2 and st < 3:
                            continue
                        col = kb * 128 if kb < 2 else 512 + (kb - 2) * 128
                        bi, bank, war = psr.next()
                        tm = None
                        for c in range(KC):
                            tm = S.op('pe', lambda e, c=c: e.matmul(
                                bank[:], lhsT=wkv[:, c, col:col + 128], rhs=hT[:, c, :],
                                start=(c == 0), stop=(c == KC - 1)),
                                deps=[tl] + tw + war if c == 0 else (), sig=(c == KC - 1))
                        ki, ks, kwar = ksr.next()
                        flip ^= 1
                        if flip:
                            te = S.op('act', lambda e: e.activation(out=ks[:], in_=bank[:], func=AF.Copy),
                                      deps=[tm] + kwar)
                        else:
                            te = S.op('dve', lambda e: e.tensor_copy(out=ks[:], in_=bank[:]), deps=[tm] + kwar)
                        psr.done(bi, [te])
                        td = S.dma('act', KT_scr[kb, :, st * 512:(st + 1) * 512], ks[:], deps=[te], stream=f"k{ki}")
                        ksr.done(ki, [td])
                        last = [tm]
                    for tt in range(4):
                        vi, vs, vwar = vsr.next()
                        tes = []
                        segs = [(256, 512, 1280), (768, 256, 1792)]
                        if st >= 3:
                            segs = [(0, 256, 256)] + segs
                        else:
                            tes.append(S.op('pool', lambda e: e.memset(vs[:, 0:256], 0.0), deps=vwar))
                        for (d0, wdt, s0) in segs:
                            bi, bank, war = psr.next()
                            tm = None
                            for c in range(KC):
                                tm = S.op('pe', lambda e, c=c: e.matmul(
                                    bank[:, 0:wdt], lhsT=hT[:, c, tt * 128:(tt + 1) * 128], rhs=wkv[:, c, s0:s0 + wdt],
                                    start=(c == 0), stop=(c == KC - 1)),
                                    deps=[tl] + tw + war if c == 0 else (), sig=(c == KC - 1))
                            flip ^= 1
                            if flip:
                                te = S.op('act', lambda e: e.activation(out=vs[:, d0:d0 + wdt], in_=bank[:, 0:wdt],
                                                                        func=AF.Copy), deps=[tm] + vwar)
                            else:
                                te = S.op('dve', lambda e: e.tensor_copy(out=vs[:, d0:d0 + wdt], in_=bank[:, 0:wdt]),
                                          deps=[tm] + vwar)
                            psr.done(bi, [te])
                            tes.append(te)
                            last = [tm]
                        r0 = st * 512 + tt * 128
                        td = S.dma('act', V_scr[r0:r0 + 128, :], vs[:], deps=tes, stream=f"v{vi}")
                        vsr.done(vi, [td])
                    hTr.done(hi, last)
                    bgc(2 if st < 7 else 4)
                S.barrier()
            if phases <= 2:
                return

            def sl(start, n, step):
                return slice(start, start + (n - 1) * step + 1, step)

            def run_pipeline(n, stage1, stage2):
                stt = {}
                for b_ in range(n + 1):
                    if b_ < n:
                        stt[b_] = stage1(b_)
                    if b_ >= 1:
                        stage2(b_ - 1, stt.pop(b_ - 1))

            with ExitStack() as pes:
                bA_b = alloc(nc, pes, "bA_b", [128, 16, 2, 128], BF16)
                bB_b = alloc(nc, pes, "bB_b", [128, 36, 2, 2, 128], BF16)
                esk = alloc(nc, pes, "esk", [64, 16], F32)
                with ExitStack() as tes_:
                    bA = alloc(nc, tes_, "bA", [128, 16, 2, 128], F32)
                    bB = alloc(nc, tes_, "bB", [128, 36, 2, 128], F32)
                    mA = alloc(nc, tes_, "mA", [128, 2, 128], F32)
                    mB = alloc(nc, tes_, "mB", [128, 2, 128], F32)
                    sk = alloc(nc, tes_, "sk", [64, 16], F32)
                    t0 = S.dma('sp', bA[:], biasA.rearrange("k g p j w q -> k (g p j) w q"), stream="c0")
                    t0 = S.dma('sp', bB[:], biasB.rearrange("k p h w q -> k (p h) w q"), stream="c0")
                    t0 = S.dma('sp', mA[:], maskA, stream="c0")
                    t0 = S.dma('sp', mB[:], maskB, stream="c0")
                    t0 = S.dma('sp', sk[:], sinks.partition_broadcast(64), stream="c0")
                    S.op('dve', lambda e: e.tensor_tensor(out=bA_b[:], in0=bA[:], in1=mA[:].unsqueeze(1).to_broadcast([128, 16, 2, 128]),
                                                          op=ALU.add), deps=[t0])
                    for u in range(2):
                        S.op('dve', lambda e: e.tensor_tensor(out=bB_b[:, :, u], in0=bB[:],
                                                              in1=mB[:].unsqueeze(1).to_broadcast([128, 36, 2, 128]), op=ALU.add))
                    S.op('act', lambda e: e.activation(out=esk[:], in_=sk[:], func=AF.Exp), deps=[t0])
                    S.barrier()
                psA = Ring(ps[0:4])
                psB = Ring(ps[4:8])
                pTr = alloc(nc, pes, "pTr", [128, 512], BF16, 6)

                with ExitStack() as aes:
                    kTAr = alloc(nc, aes, "kTA", [128, 17 * 128], BF16, 2)
                    qTAr = alloc(nc, aes, "qTA", [128, 2, 2, T], BF16, 2)
                    for q_ in qTAr.tiles:
                        S.op('pool', lambda e: e.memset(q_[:], 0.0))
                    VAr = alloc(nc, aes, "VA", [128, 17, 64], BF16, 2)
                    oAr = alloc(nc, aes, "oA", [64, 4, T], BF16, 2)
                    dsb = alloc(nc, aes, "dsb", [64, 512], F32, 2)

                    def loadA(g):
                        kb, ko = g // 2, (g % 2) * 64
                        ki, kTA, kwar = kTAr.next()
                        _, qTA, qwar = qTAr.next()
                        _, VA, vwar = VAr.next()
                        tl = None
                        for half in range(2):
                            tl = S.dma('sp', kTA[half * 64:(half + 1) * 64, :], KT_scr[kb, ko:ko + 64, T - 128:2 * T],
                                       deps=kwar + qwar + vwar, stream=f"a{ki}")
                        for par_ in range(2):
                            tl = S.dma('sp', qTA[par_ * 64:(par_ + 1) * 64, par_, :, :],
                                       QT_scr[2 * g:2 * g + 2, par_ * 64:(par_ + 1) * 64, :].rearrange("b p t -> p b t"),
                                       deps=[('e', 'pool', S.cnt['pool'])], stream=f"a{ki}")
                        tl = S.dma('sp', VA[:], V_scr[T - 128:2 * T, g * 64:(g + 1) * 64].rearrange("(i p) c -> p i c", p=128),
                                   stream=f"a{ki}")
                        return ki, kTA, qTA, VA, [tl]

                    nxt = loadA(0)
                    for g in range(4):
                        ki, kTA, qTA, VA, tl = nxt
                        if g + 1 < 4:
                            nxt = loadA(g + 1)
                        oi, oA, owar = oAr.next()
                        lastpe = [None]
                        lastf3 = [None]

                        def a_stage1(i):
                            pts = []
                            for par in range(2):
                                o = par * 64
                                bi, bank, war = psA.next()
                                S.op('pe', lambda e: e.matmul(bank[:], lhsT=identb[:],
                                                              rhs=bA_b[:, g * 4 + par * 2:g * 4 + par * 2 + 2].rearrange("k j w q -> k (j w q)"),
                                                              start=True, stop=False), deps=tl + war, sig=False)
                                tm = None
                                n = 0
                                for jj in range(2):
                                    for w in range(2):
                                        kt = i + w
                                        tm = S.op('pe', lambda e: e.matmul(
                                            bank[:, (jj * 2 + w) * 128:(jj * 2 + w + 1) * 128],
                                            lhsT=kTA[:, kt * 128:(kt + 1) * 128],
                                            rhs=qTA[:, par, jj, i * 128:(i + 1) * 128], start=False, stop=(n == 3)), sig=(n == 3))
                                        n += 1
                                pi, pT, pwar = pTr.next()
                                te = S.op('act', lambda e: e.activation(out=pT[:], in_=bank[:], func=AF.Exp), deps=[tm] + pwar)
                                psA.done(bi, [te])
                                pts.append((pi, pT, te))
                            return pts

                        def a_stage2(i, pts):
                            nbi, nbank, nwar = psB.next()
                            dbi, dbank, dwar = psB.next()
                            tn = td_ = None
                            for par in range(2):
                                pi, pT, te = pts[par]
                                pv = pT[:].rearrange("k (j w q) -> k j w q", j=2, w=2)
                                for w in range(2):
                                    kt = i + w
                                    tn = S.op('pe', lambda e: e.matmul(
                                        nbank[0:64, par * 256:(par + 1) * 256], lhsT=VA[:, kt, :], rhs=pv[:, :, w, :],
                                        start=(w == 0), stop=(w == 1)), deps=[te] + nwar, sig=(par == 1 and w == 1))
                            for par in range(2):
                                pi, pT, te = pts[par]
                                pv = pT[:].rearrange("k (j w q) -> k j w q", j=2, w=2)
                                for w in range(2):
                                    lh = onesh_b[:, :] if (i == 0 and w == 0) else ones_b[:, 0:64]
                                    td_ = S.op('pe', lambda e: e.matmul(
                                        dbank[0:64, par * 256:(par + 1) * 256], lhsT=lh, rhs=pv[:, :, w, :],
                                        start=(w == 0), stop=(w == 1)), deps=[te] + dwar, sig=(par == 1 and w == 1))
                            for par in range(2):
                                pTr.done(pts[par][0], [td_])
                            di, ds, dswar = dsb.next()
                            f1 = S.op('dve', lambda e: e.tensor_tensor(
                                out=ds[:].rearrange("p (h q) -> p h q", h=4), in0=dbank[0:64, :].rearrange("p (h q) -> p h q", h=4),
                                in1=esk[:, g * 4:(g + 1) * 4].unsqueeze(2).to_broadcast([64, 4, 128]), op=ALU.add),
                                deps=[td_] + dswar)
                            f2 = S.op('dve', lambda e: e.reciprocal(out=ds[:], in_=ds[:]))
                            f3 = S.op('dve', lambda e: e.tensor_tensor(
                                out=oA[:, :, i * 128:(i + 1) * 128], in0=nbank[0:64, :].rearrange("p (h q) -> p h q", h=4),
                                in1=ds[:].rearrange("p (h q) -> p h q", h=4), op=ALU.mult), deps=[tn] + (owar if i == 0 else []))
                            psB.done(nbi, [f3])
                            psB.done(dbi, [f1])
                            dsb.done(di, [f3])
                            lastpe[0] = td_
                            lastf3[0] = f3

                        run_pipeline(NT, a_stage1, a_stage2)
                        kTAr.done(ki, [lastpe[0]])
                        qTAr.done(ki, [lastpe[0]])
                        VAr.done(ki, [lastpe[0]])
                        tds = []
                        for par in range(2):
                            for jj in range(2):
                                tds.append(S.dma('pool', OT_scr[2 * g + jj, par * 64:(par + 1) * 64, :], oA[:, par * 2 + jj, :],
                                                 deps=[lastf3[0]], stream=f"o{oi}"))
                        oAr.done(oi, [tds[-1]])
                        bgc(3)
                    S.barrier()

                with ExitStack() as bes:
                    kTBr = alloc(nc, bes, "kTB", [128, 2 * T], BF16, 2)
                    qTBr = alloc(nc, bes, "qTB", [128, 2, T], BF16, 2)
                    for q_ in qTBr.tiles:
                        S.op('pool', lambda e: e.memset(q_[:], 0.0))
                    VBr = alloc(nc, bes, "VB", [128, 32, 128], BF16, 2)
                    accnr = alloc(nc, bes, "accn", [64, 2, T], F32, 2)
                    accdr = alloc(nc, bes, "accd", [64, 2, T], F32, 2)
                    oBr = alloc(nc, bes, "oB", [64, 2, T], BF16, 2)

                    def loadB(hp):
                        ki, kTB, kwar = kTBr.next()
                        _, qTB, qwar = qTBr.next()
                        S.dma('sp', kTB[:], KT_scr[2 + hp], deps=kwar + qwar, stream=f"a{ki}")
                        for par_ in range(2):
                            tl = S.dma('sp', qTB[par_ * 64:(par_ + 1) * 64, par_, :], QT_scr[8 + hp, par_ * 64:(par_ + 1) * 64, :],
                                       deps=[('e', 'pool', S.cnt['pool'])], stream=f"a{ki}")
                        return ki, kTB, qTB, [tl]

                    nxt = loadB(0)
                    for hp in range(6):
                        ki, kTB, qTB, tl = nxt
                        if hp + 1 < 6:
                            nxt = loadB(hp + 1)
                        ai, accn, anwar = accnr.next()
                        _, accd, adwar = accdr.next()
                        lastpe = [None]
                        lastacc = [None, None]
                        for p, r in enumerate((1, 4, 16)):
                            span = 128 * r
                            nbh = T // span
                            vi, VB, vwar = VBr.next()
                            VBv = VB[:].rearrange("p (b r) c -> p b r c", r=r)
                            tv = []
                            for blk in range(nbh - 1, 2 * nbh):
                                srcv = V_scr[blk * span:(blk + 1) * span, 256 + hp * 128:256 + (hp + 1) * 128].rearrange(
                                    "(j r) c -> j r c", r=r)
                                tv.append(S.dma('sp', VBv[:, blk, :, :], srcv, deps=vwar, stream=f"vb{vi}"))
                            tv = [tv[-1]]
                            lastpv = [None]

                            def b_stage1(bt):
                                tiles = [((bt * 2 + u) // r, (bt * 2 + u) % r) for u in range(2)]
                                pts = []
                                for par in range(2):
                                    o = par * 64
                                    bi, bank, war = psA.next()
                                    S.op('pe', lambda e: e.matmul(bank[:], lhsT=identb[:],
                                                                  rhs=bB_b[:, p * 12 + hp * 2 + par].rearrange("k u w q -> k (u w q)"),
                                                                  start=True, stop=False), deps=tl + war, sig=False)
                                    tm = None
                                    n = 0
                                    for u, (blk, res) in enumerate(tiles):
                                        for w in range(2):
                                            kstart = (blk + nbh - 1 + w) * span + res
                                            qstart = blk * span + res
                                            tm = S.op('pe', lambda e: e.matmul(
                                                bank[:, (u * 2 + w) * 128:(u * 2 + w + 1) * 128],
                                                lhsT=kTB[:, sl(kstart, 128, r)],
                                                rhs=qTB[:, par, sl(qstart, 128, r)], start=False, stop=(n == 3)), sig=(n == 3))
                                            n += 1
                                    pi, pT, pwar = pTr.next()
                                    te = S.op('act', lambda e: e.activation(out=pT[:], in_=bank[:], func=AF.Exp), deps=[tm] + pwar)
                                    psA.done(bi, [te])
                                    pts.append((pi, pT, te))
                                return tiles, pts

                            def b_stage2(bt, stt_):
                                tiles, pts = stt_
                                nbi, nbank, nwar = psB.next()
                                dbi, dbank, dwar = psB.next()
                                tn = td_ = None
                                for par in range(2):
                                    pi, pT, te = pts[par]
                                    for u, (blk, res) in enumerate(tiles):
                                        for w in range(2):
                                            vt = (blk + nbh - 1 + w) * r + res
                                            tn = S.op('pe', lambda e: e.matmul(
                                                nbank[0:64, (par * 2 + u) * 128:(par * 2 + u + 1) * 128],
                                                lhsT=VB[:, vt, par * 64:(par + 1) * 64],
                                                rhs=pT[:, (u * 2 + w) * 128:(u * 2 + w + 1) * 128],
                                                start=(w == 0), stop=(w == 1)), deps=[te] + nwar + tv,
                                                sig=(par == 1 and u == 1 and w == 1))
                                for par in range(2):
                                    pi, pT, te = pts[par]
                                    for u, (blk, res) in enumerate(tiles):
                                        for w in range(2):
                                            lh = onesh_b[:, :] if (blk == 0 and w == 0) else ones_b[:, 0:64]
                                            td_ = S.op('pe', lambda e: e.matmul(
                                                dbank[0:64, (par * 2 + u) * 128:(par * 2 + u + 1) * 128],
                                                lhsT=lh, rhs=pT[:, (u * 2 + w) * 128:(u * 2 + w + 1) * 128],
                                                start=(w == 0), stop=(w == 1)), deps=[te] + dwar,
                                                sig=(par == 1 and u == 1 and w == 1))
                                for par in range(2):
                                    pTr.done(pts[par][0], [td_])

                                def dst(acc):
                                    (b0, r0), (b1, r1) = tiles
                                    if r == 1:
                                        return acc[:, :, b0 * 128:(b0 + 2) * 128].rearrange("p a (u j) -> p a u j", u=2)
                                    v_ = acc[:, :, b0 * span:(b0 + 1) * span].rearrange("p a (j r) -> p a r j", r=r)
                                    return v_[:, :, r0:r0 + 2, :]
                                srcn = nbank[0:64, :].rearrange("p (a u q) -> p a u q", a=2, u=2)
                                srcd = dbank[0:64, :].rearrange("p (a u q) -> p a u q", a=2, u=2)
                                if p == 0:
                                    a1 = S.op('act', lambda e: e.activation(out=dst(accn), in_=srcn, func=AF.Copy),
                                              deps=[tn] + anwar)
                                    a2 = S.op('act', lambda e: e.activation(out=dst(accd), in_=srcd, func=AF.Copy),
                                              deps=[td_] + adwar)
                                else:
                                    a1 = S.op('dve', lambda e: e.tensor_tensor(out=dst(accn), in0=srcn, in1=dst(accn), op=ALU.add),
                                              deps=[tn, lastacc[0], lastacc[1]])
                                    a2 = S.op('dve', lambda e: e.tensor_tensor(out=dst(accd), in0=srcd, in1=dst(accd), op=ALU.add),
                                              deps=[td_])
                                psB.done(nbi, [a1])
                                psB.done(dbi, [a2])
                                lastpv[0] = tn
                                lastpe[0] = td_
                                if p == 0:
                                    lastacc[0], lastacc[1] = a1, a2
                                else:
                                    lastacc[0], lastacc[1] = a1, a2

                            run_pipeline(8, b_stage1, b_stage2)
                            VBr.done(vi, [lastpv[0]])
                            bgc(3 if hp < 4 else 0)
                        kTBr.done(ki, [lastpe[0]])
                        qTBr.done(ki, [lastpe[0]])
                        oi, oB, owar = oBr.next()
                        f1 = S.op('dve', lambda e: e.reciprocal(out=accd[:], in_=accd[:]), deps=[lastacc[0], lastacc[1]])
                        f2 = S.op('pool', lambda e: e.tensor_tensor(out=oB[:], in0=accn[:], in1=accd[:], op=ALU.mult),
                                  deps=[f1, lastacc[0], lastacc[1]] + owar)
                        accnr.done(ai, [f2])
                        accdr.done(ai, [f2])
                        tds = []
                        for par in range(2):
                            tds.append(S.dma('pool', OT_scr[8 + hp, par * 64:(par + 1) * 64, :], oB[:, par, :], deps=[f2],
                                             stream=f"o{oi}"))
                        oBr.done(oi, [tds[-1]])
                    S.barrier()
            if phases <= 2.5:
                return

            with ExitStack() as pes:
                hT = alloc(nc, pes, "hT3", [128, KC, T], BF16)
                OT = alloc(nc, pes, "OT3", [128, 14, T], BF16)
                bg = alloc(nc, pes, "bg", [128, 32], F32)
                tl = [S.dma('sp', bg[:], b_gate, stream="c0")]
                for st in range(4):
                    tl.append(S.dma('sp', hT[:, :, st * 512:(st + 1) * 512], hT_scr[:, :, T + st * 512:T + (st + 1) * 512], stream="c0"))
                    tl.append(S.dma('sp', OT[:, :, st * 512:(st + 1) * 512],
                                    OT_scr[:, :, st * 512:(st + 1) * 512].rearrange("b p t -> p b t"), stream="c0"))
                tl = [tl[-1]]
                wr = alloc(nc, pes, "w3r", [128, 46, 128], BF16, 3)
                sgr = alloc(nc, pes, "sgr", [128, 512], F32, 4)
                zsr = alloc(nc, pes, "zsr", [128, 512], BF16, 3)
                wgv = w_gate.rearrange("(c p) n -> p c n", p=128)
                wav = w_a_out.rearrange("(c p) n -> p c n", p=128)
                wbv = w_b_out.rearrange("(c p) n -> p c n", p=128)
                def load3(fb_):
                    wi_, wt_, wwar_ = wr.next()
                    f0 = fb_ * 128
                    S.dma('pool', wt_[:, 0:16, :], wgv[:, :, f0:f0 + 128], deps=wwar_, stream=f"w{wi_}")
                    S.dma('pool', wt_[:, 16:32, :], wgv[:, :, D + f0:D + f0 + 128], stream=f"w{wi_}")
                    S.dma('pool', wt_[:, 32:40, :], wav[:, :, f0:f0 + 128], stream=f"w{wi_}")
                    tw_ = [S.dma('pool', wt_[:, 40:46, :], wbv[:, :, f0:f0 + 128], stream=f"w{wi_}")]
                    return wi_, wt_, tw_

                nxt3 = [load3(0), load3(1)]
                for fb in range(KC):
                    wi, wt, tw = nxt3.pop(0)
                    if fb + 2 < KC:
                        nxt3.append(load3(fb + 2))
                    lastpe = None
                    for st in range(4):
                        tsl = slice(st * 512, (st + 1) * 512)
                        banks = []
                        for (w0, nk, src, s0) in ((0, 16, hT, 0), (16, 16, hT, 0), (32, 8, OT, 0), (40, 6, OT, 8)):
                            bi, bank, war = psr.next()
                            tm = None
                            for c in range(nk):
                                tm = S.op('pe', lambda e: e.matmul(
                                    bank[:], lhsT=wt[:, w0 + c, :], rhs=src[:, s0 + c, tsl],
                                    start=(c == 0), stop=(c == nk - 1)),
                                    deps=tl + tw + war if c == 0 else (), sig=(c == nk - 1))
                            banks.append((bi, bank, tm))
                            lastpe = tm
                        ai, sga, awar = sgr.next()
                        bi2, sgb, bwar = sgr.next()
                        ta = S.op('act', lambda e: e.activation(out=sga[:], in_=banks[0][1][:], func=AF.Sigmoid,
                                                                bias=bg[:, fb:fb + 1], scale=1.0), deps=[banks[0][2]] + awar)
                        tb = S.op('act', lambda e: e.activation(out=sgb[:], in_=banks[1][1][:], func=AF.Sigmoid,
                                                                bias=bg[:, 16 + fb:17 + fb], scale=1.0), deps=[banks[1][2]] + bwar)
                        psr.done(banks[0][0], [ta])
                        psr.done(banks[1][0], [tb])
                        m1 = S.op('dve', lambda e: e.tensor_tensor(out=sga[:], in0=banks[2][1][:], in1=sga[:], op=ALU.mult),
                                  deps=[ta, banks[2][2]])
                        m2 = S.op('dve', lambda e: e.tensor_tensor(out=sgb[:], in0=banks[3][1][:], in1=sgb[:], op=ALU.mult),
                                  deps=[tb, banks[3][2]])
                        psr.done(banks[2][0], [m1])
                        psr.done(banks[3][0], [m2])
                        zi, zs, zwar = zsr.next()
                        m3 = S.op('pool', lambda e: e.tensor_tensor(out=zs[:], in0=sga[:], in1=sgb[:], op=ALU.add),
                                  deps=[m1, m2] + zwar)
                        sgr.done(ai, [m3])
                        sgr.done(bi2, [m3])
                        td = S.dma('sp', zT_scr[fb, :, tsl], zs[:], deps=[m3], stream=f"z{zi}")
                        zsr.done(zi, [td])
                    wr.done(wi, [lastpe])
                S.barrier()
            if phases <= 3:
                return

            with ExitStack() as pes:
                wo = alloc(nc, pes, "wo", [128, KC, D], BF16)
                wov = w_o.rearrange("(c p) n -> p c n", p=128)
                tw = []
                for c4 in range(4):
                    for hf in range(2):
                        tw.append(S.dma('pool', wo[:, c4 * 4:(c4 + 1) * 4, hf * 1024:(hf + 1) * 1024],
                                        wov[:, c4 * 4:(c4 + 1) * 4, hf * 1024:(hf + 1) * 1024], stream="w0"))
                tw = [tw[-1]]
                zr = alloc(nc, pes, "zr", [128, KC, 512], BF16, 2)
                xr = alloc(nc, pes, "x4r", [128, D], F32, 3)
                for st in range(4):
                    zi, zT, zwar = zr.next()
                    tz = S.dma('sp', zT[:], zT_scr[:, :, st * 512:(st + 1) * 512].rearrange("c p t -> p c t"), deps=zwar, stream=f"h{zi}")
                    lastpe = None
                    for tt in range(4):
                        r0 = st * 512 + tt * 128
                        xi, xt, xwar = xr.next()
                        tx = S.dma('sp', xt[:], xo[r0:r0 + 128, :], deps=xwar, stream=f"x{xi}")
                        tas = []
                        for cb in range(4):
                            bi, bank, war = psr.next()
                            tm = None
                            for c in range(KC):
                                tm = S.op('pe', lambda e: e.matmul(
                                    bank[:], lhsT=zT[:, c, tt * 128:(tt + 1) * 128], rhs=wo[:, c, cb * 512:(cb + 1) * 512],
                                    start=(c == 0), stop=(c == KC - 1)),
                                    deps=[tz] + tw + war if c == 0 else (), sig=(c == KC - 1))
                            ta = S.op('dve', lambda e: e.tensor_tensor(out=xt[:, cb * 512:(cb + 1) * 512], in0=bank[:],
                                                                       in1=xt[:, cb * 512:(cb + 1) * 512], op=ALU.add), deps=[tm, tx])
                            psr.done(bi, [ta])
                            tas.append(ta)
                            lastpe = tm
                        td = S.dma('act', x1_scr[r0:r0 + 128, :], xt[:], deps=tas, stream=f"x{xi}")
                        xr.done(xi, [td])
                    zr.done(zi, [lastpe])
                S.barrier()
            if phases <= 4:
                return

            slots_i = alloc(nc, es, "slots_i", [128, 32], I32)
            gates_f = alloc(nc, es, "gates_f", [128, 32], F32)
            with ExitStack() as pes:
                gx_bc = alloc(nc, pes, "gx_bc", [128, D], F32)
                gmoe_bc = alloc(nc, pes, "gmoe_bc", [128, D], F32)
                wxq = alloc(nc, pes, "wxq", [128, KC, 512], BF16)
                wxo = alloc(nc, pes, "wxo", [128, 4, D], BF16)
                wr_sb = alloc(nc, pes, "wr_sb", [128, KC, 36], F32)
                KxT = alloc(nc, pes, "KxT", [128, 4, 256], BF16)
                Vx = alloc(nc, pes, "Vx", [128, 2, 512], BF16)
                Umat = alloc(nc, pes, "Umat", [128, 128], BF16)
                eC = alloc(nc, pes, "eC", [128, 32], F32)
                eCi = alloc(nc, pes, "eCi", [128, 32], I32)
                TRf = alloc(nc, pes, "TRf", [128, 1], F32)
                TRi = alloc(nc, pes, "TRi", [128, 1], I32)
                cum = alloc(nc, pes, "cum", [128, 32], F32)
                junk = alloc(nc, pes, "junk4", [128, D], BF16)
                ssm = alloc(nc, pes, "ssm", [128, 8], F32)
                tset = []
                tset.append(S.dma('sp', gx_bc[:], g_x.partition_broadcast(128), stream="c0"))
                tset.append(S.dma('sp', gmoe_bc[:], g_moe.partition_broadcast(128), stream="c0"))
                tset.append(S.dma('sp', wr_sb[:], w_r.rearrange("(c p) n -> p c n", p=128), stream="c0"))
                tset.append(S.dma('pool', wxq[:], w_xq.rearrange("(c p) n -> p c n", p=128), stream="w0"))
                tset.append(S.dma('pool', wxo[:], w_xo.rearrange("(c p) n -> p c n", p=128), stream="w0"))
                tset.append(S.op('pool', lambda e: e.memset(Umat[:], 1.0)))
                tset.append(S.op('pool', lambda e: e.affine_select(out=Umat[:], in_=Umat[:], pattern=[[1, 128]], base=0,
                                                                   channel_multiplier=-1, compare_op=ALU.is_gt, fill=0.0)))
                tset.append(S.op('pool', lambda e: e.iota(eCi[:], pattern=[[CAP, 32]], base=0, channel_multiplier=0)))
                tset.append(S.op('pool', lambda e: e.tensor_copy(out=eC[:], in_=eCi[:]), ss=True))
                tset.append(S.op('pool', lambda e: e.iota(TRi[:], pattern=[[0, 1]], base=NEXP * CAP, channel_multiplier=1)))
                tset.append(S.op('pool', lambda e: e.tensor_copy(out=TRf[:], in_=TRi[:]), ss=True))
                tset.append(S.op('pool', lambda e: e.memset(cum[:], 0.0)))
                with ExitStack() as mes:
                    gm_bc = alloc(nc, mes, "gm_bc", [128, D], F32)
                    wxk = alloc(nc, mes, "wxk", [128, KC, 512], BF16)
                    wxv = alloc(nc, mes, "wxv", [128, KC, 512], BF16)
                    mt_ = alloc(nc, mes, "mt_", [128, D], F32, 2)
                    mb_ = alloc(nc, mes, "mb_", [128, D], BF16, 2)
                    mT = alloc(nc, mes, "mT", [128, KC, 256], BF16)
                    t1_ = S.dma('sp', gm_bc[:], g_mem.partition_broadcast(128), stream="c0")
                    t2_ = S.dma('pool', wxk[:], w_xk.rearrange("(c p) n -> p c n", p=128), stream="w1")
                    t3_ = S.dma('pool', wxv[:], w_xv.rearrange("(c p) n -> p c n", p=128), stream="w1")
                    tks = []
                    for m in range(2):
                        _, xt, _ = mt_.next()
                        _, hb, _ = mb_.next()
                        tl = S.dma('sp', xt[:], memb[m * 128:(m + 1) * 128, :], stream="c0")
                        t4, _t = rmsnorm_tile(xt[:], gm_bc[:], hb[:], ssm[:, m:m + 1], ssm[:, 2 + m:3 + m], ssm[:, 4 + m:5 + m],
                                              junk, [tl, t1_])
                        tks += transpose_tile(hb, mT[:, :, m * 128:(m + 1) * 128], [t4], [])
                    evs = []
                    for hd in range(4):
                        bi, bank, war = psr.next()
                        tm = None
                        for c in range(KC):
                            tm = S.op('pe', lambda e: e.matmul(bank[:, 0:256], lhsT=wxk[:, c, hd * 128:(hd + 1) * 128], rhs=mT[:, c, :],
                                                               start=(c == 0), stop=(c == KC - 1)),
                                      deps=tks + [t3_] + war if c == 0 else (), sig=(c == KC - 1))
                        te = S.op('act', lambda e: e.activation(out=KxT[:, hd, :], in_=bank[:, 0:256], func=AF.Copy), deps=[tm])
                        psr.done(bi, [te])
                        evs.append(te)
                    for m in range(2):
                        bi, bank, war = psr.next()
                        tm = None
                        for c in range(KC):
                            tm = S.op('pe', lambda e: e.matmul(bank[:], lhsT=mT[:, c, m * 128:(m + 1) * 128], rhs=wxv[:, c, :],
                                                               start=(c == 0), stop=(c == KC - 1)),
                                      deps=tks + [t3_] + war if c == 0 else (), sig=(c == KC - 1))
                        te = S.op('dve', lambda e: e.tensor_copy(out=Vx[:, m, :], in_=bank[:]), deps=[tm])
                        psr.done(bi, [te])
                        evs.append(te)
                    tset += evs
                    S.barrier()

                xr = alloc(nc, pes, "x5r", [128, D], F32, 2)
                hbr = alloc(nc, pes, "hb5", [128, D], BF16, 2)
                h2Tr = alloc(nc, pes, "h2T", [128, KC, 128], BF16, 2)
                qTr = alloc(nc, pes, "qT5", [128, 4, 128], BF16, 2)
                pTr = alloc(nc, pes, "pT5", [128, 8, 128], BF16, 2)
                rdr = alloc(nc, pes, "rd5", [128, 512], F32, 2)
                oxr = alloc(nc, pes, "ox5", [128, 4, 128], BF16, 2)
                h3fr = alloc(nc, pes, "h3f", [128, D], F32, 2)
                h3br = alloc(nc, pes, "h3b", [128, D], BF16, 2)
                h3Tr = alloc(nc, pes, "h3T", [128, KC, 128], F32, 2)
                ss5 = alloc(nc, pes, "ss5", [128, 96], F32)
                rt = alloc(nc, pes, "rt", [128, 512], F32)
                indb = alloc(nc, pes, "indb", [128, 32], BF16, 2)
                xscale = 128.0 ** -0.5
                cum_tok = []
                cum_tok_ = [[]]
                def p4_h1(t):
                        r0 = t * 128
                        xi, xt, xwar = xr.next()
                        tx = S.dma('sp', xt[:], x1_scr[r0:r0 + 128, :], deps=xwar, stream=f"x{xi}")
                        hi, hb, hwar = hbr.next()
                        t4, _t = rmsnorm_tile(xt[:], gx_bc[:], hb[:], ss5[:, t:t + 1], ss5[:, 16 + t:17 + t], ss5[:, 32 + t:33 + t],
                                              junk, [tx] + tset + hwar)
                        h2i, h2T, h2war = h2Tr.next()
                        tks = transpose_tile(hb, h2T[:, :, :], [t4], h2war)
                        hbr.done(hi, tks)
                        bi, bank, war = psr.next()
                        tm = None
                        for hd in range(4):
                            for c in range(KC):
                                tm = S.op('pe', lambda e: e.matmul(bank[:, hd * 128:(hd + 1) * 128], lhsT=wxq[:, c, hd * 128:(hd + 1) * 128],
                                                                   rhs=h2T[:, c, :], start=(c == 0), stop=(c == KC - 1)),
                                          deps=tks + war + tset if (c == 0 and hd == 0) else (), sig=(c == KC - 1 and hd == 3))
                        h2Tr.done(h2i, [tm])
                        qi, qT, qwar = qTr.next()
                        tq = S.op('act', lambda e: e.activation(out=qT[:].rearrange("p h t -> p (h t)"), in_=bank[:], func=AF.Copy, scale=xscale),
                                  deps=[tm] + qwar)
                        psr.done(bi, [tq])
                        pi, pT, pwar = pTr.next()
                        tes = []
                        tm2 = None
                        for hh2 in range(2):
                            bi, bank, war = psr.next()
                            for hl in range(2):
                                hd = hh2 * 2 + hl
                                for m in range(2):
                                    tm2 = S.op('pe', lambda e: e.matmul(bank[:, (hl * 2 + m) * 128:(hl * 2 + m + 1) * 128],
                                                                        lhsT=KxT[:, hd, m * 128:(m + 1) * 128], rhs=qT[:, hd, :],
                                                                        start=True, stop=True),
                                               deps=[tq] + war, sig=(hl == 1 and m == 1))
                            te = S.op('act', lambda e: e.activation(out=pT[:, hh2 * 4:(hh2 + 1) * 4, :].rearrange("p a t -> p (a t)"),
                                                                    in_=bank[:], func=AF.Exp), deps=[tm2] + pwar)
                            psr.done(bi, [te])
                            tes.append(te)
                        qTr.done(qi, [tm2])
                        nbi, nbank, nwar = psr.next()
                        dbi, dbank, dwar = psr.next()
                        tn = td_ = None
                        for hd in range(4):
                            for m in range(2):
                                tn = S.op('pe', lambda e: e.matmul(nbank[:, hd * 128:(hd + 1) * 128], lhsT=Vx[:, m, hd * 128:(hd + 1) * 128],
                                                                   rhs=pT[:, hd * 2 + m, :], start=(m == 0), stop=(m == 1)),
                                          deps=tes + nwar, sig=(hd == 3 and m == 1))
                        for hd in range(4):
                            for m in range(2):
                                td_ = S.op('pe', lambda e: e.matmul(dbank[:, hd * 128:(hd + 1) * 128], lhsT=ones_b[:],
                                                                    rhs=pT[:, hd * 2 + m, :], start=(m == 0), stop=(m == 1)),
                                           deps=tes + dwar, sig=(hd == 3 and m == 1))
                        pTr.done(pi, [td_])
                        ri, rd, rwar = rdr.next()
                        f1 = S.op('dve', lambda e: e.reciprocal(out=rd[:], in_=dbank[:]), deps=[td_] + rwar)
                        oi, ox, owar = oxr.next()
                        f2 = S.op('dve', lambda e: e.tensor_tensor(out=ox[:].rearrange("p h t -> p (h t)"), in0=nbank[:], in1=rd[:], op=ALU.mult),
                                  deps=[tn] + owar)
                        psr.done(dbi, [f1])
                        psr.done(nbi, [f2])
                        rdr.done(ri, [f2])
                        tas = []
                        tm3 = None
                        for cb in range(4):
                            bi, bank, war = psr.next()
                            for hd in range(4):
                                tm3 = S.op('pe', lambda e: e.matmul(bank[:], lhsT=ox[:, hd, :], rhs=wxo[:, hd, cb * 512:(cb + 1) * 512],
                                                                    start=(hd == 0), stop=(hd == 3)), deps=[f2] + war, sig=(hd == 3))
                            ta = S.op('dve', lambda e: e.tensor_tensor(out=xt[:, cb * 512:(cb + 1) * 512], in0=bank[:],
                                                                       in1=xt[:, cb * 512:(cb + 1) * 512], op=ALU.add), deps=[tm3, t4, _t])
                            psr.done(bi, [ta])
                            tas.append(ta)
                        oxr.done(oi, [tm3])
                        tdx = S.dma('sp', x2_scr[r0:r0 + 128, :], xt[:], deps=tas, stream=f"x{xi}")
                        return dict(xi=xi, xt=xt, tas=tas, tdx=tdx, r0=r0)

                def p4_h2(t, st_):
                        xi, xt, tas, tdx = st_["xi"], st_["xt"], st_["tas"], st_["tdx"]
                        cum_tok = cum_tok_[0]
                        fi, h3f, fwar = h3fr.next()
                        t5, _t5 = rmsnorm_tile(xt[:], gmoe_bc[:], h3f[:], ss5[:, 48 + t:49 + t], ss5[:, 64 + t:65 + t], ss5[:, 80 + t:81 + t],
                                               junk, tas + fwar)
                        xr.done(xi, [tdx, t5, _t5])
                        b3i, h3b, b3war = h3br.next()
                        tcb = S.op('act', lambda e: e.activation(out=h3b[:], in_=h3f[:], func=AF.Copy), deps=[t5] + b3war)
                        h3i, h3T, h3war = h3Tr.next()
                        tcs = []
                        tp = None
                        for q4 in range(4):
                            bi, bank, war = psr.next()
                            for j in range(4):
                                c = q4 * 4 + j
                                tp = S.op('pe', lambda e: e.transpose(out=bank[:, j * 128:(j + 1) * 128], in_=h3f[:, c * 128:(c + 1) * 128],
                                                                      identity=identf[:]), deps=[t5] + war, sig=(j == 3))
                            dst_ = h3T[:, q4 * 4:(q4 + 1) * 4, :].rearrange("p c t -> p (c t)")
                            if q4 % 2 == 0:
                                tc_ = S.op('act', lambda e: e.activation(out=dst_, in_=bank[:], func=AF.Copy), deps=[tp] + h3war)
                            else:
                                tc_ = S.op('dve', lambda e: e.tensor_copy(out=dst_, in_=bank[:]), deps=[tp] + h3war)
                            psr.done(bi, [tc_])
                            tcs.append(tc_)
                        h3fr.done(fi, [tp, tcb])
                        bi, bank, war = psr.next()
                        tml = None
                        for c in range(KC):
                            tml = S.op('pe', lambda e: e.matmul(bank[:, 0:36], lhsT=h3T[:, c, :], rhs=wr_sb[:, c, :],
                                                                start=(c == 0), stop=(c == KC - 1)), deps=tcs + war + tset, sig=(c == KC - 1))
                        h3Tr.done(h3i, [tml])
                        L = rt[:, 0:36]
                        gmax, ngmax, gsum, ggate = rt[:, 40:41], rt[:, 41:42], rt[:, 42:43], rt[:, 43:44]
                        goh, pen, j4 = rt[:, 44:48], rt[:, 48:52], rt[:, 52:56]
                        em, oh1, em2, oh2 = rt[:, 64:96], rt[:, 96:128], rt[:, 128:160], rt[:, 160:192]
                        m1, m2, d21, e21, w1_, w2_ = rt[:, 192:193], rt[:, 193:194], rt[:, 194:195], rt[:, 195:196], rt[:, 196:197], rt[:, 197:198]
                        posf, slotv, prod = rt[:, 224:256], rt[:, 256:288], rt[:, 288:320]
                        s1, p1, ov, tmp1 = rt[:, 320:321], rt[:, 321:322], rt[:, 322:323], rt[:, 323:324]
                        V_ = lambda fn, deps=(): S.op('dve', fn, deps=deps, ss=True)
                        tL = V_(lambda e: e.tensor_copy(out=L, in_=bank[:, 0:36]), deps=[tml] + cum_tok)
                        psr.done(bi, [tL])
                        V_(lambda e: e.tensor_reduce(out=gmax, in_=rt[:, 0:4], axis=AX.X, op=ALU.max))
                        V_(lambda e: e.tensor_scalar(out=ngmax, in0=gmax, scalar1=-1.0, scalar2=None, op0=ALU.mult))
                        tg1 = V_(lambda e: e.tensor_scalar(out=goh, in0=rt[:, 0:4], scalar1=gmax, scalar2=None, op0=ALU.is_equal))
                        tg2 = S.op('act', lambda e: e.activation(out=j4, in_=rt[:, 0:4], func=AF.Exp, bias=ngmax, scale=1.0, accum_out=gsum),
                                   deps=[tg1])
                        V_(lambda e: e.tensor_scalar(out=pen, in0=goh, scalar1=1.0, scalar2=1e30, op0=ALU.subtract, op1=ALU.mult))
                        V_(lambda e: e.tensor_tensor(out=em.rearrange("p (g x) -> p g x", g=4), in0=rt[:, 4:36].rearrange("p (g x) -> p g x", g=4),
                                                     in1=pen.unsqueeze(2).to_broadcast([128, 4, 8]), op=ALU.add))
                        V_(lambda e: e.tensor_reduce(out=m1, in_=em, axis=AX.X, op=ALU.max))
                        V_(lambda e: e.tensor_scalar(out=oh1, in0=em, scalar1=m1, scalar2=None, op0=ALU.is_equal))
                        V_(lambda e: e.scalar_tensor_tensor(out=em2, in0=oh1, scalar=-1e30, in1=em, op0=ALU.mult, op1=ALU.add))
                        V_(lambda e: e.tensor_reduce(out=m2, in_=em2, axis=AX.X, op=ALU.max))
                        V_(lambda e: e.tensor_scalar(out=oh2, in0=em2, scalar1=m2, scalar2=None, op0=ALU.is_equal))
                        td21 = V_(lambda e: e.tensor_tensor(out=d21, in0=m2, in1=m1, op=ALU.subtract))
                        te21 = S.op('act', lambda e: e.activation(out=e21, in_=d21, func=AF.Exp), deps=[td21])
                        ii, ind, iwar = indb.next()
                        tind = V_(lambda e: e.tensor_tensor(out=ind[:], in0=oh1, in1=oh2, op=ALU.add), deps=iwar)
                        bi, bank, war = psr.next()
                        S.op('pe', lambda e: e.matmul(bank[:, 0:32], lhsT=Umat[:], rhs=ind[:], start=True, stop=True), deps=[tind] + war + tset, sig=False)
                        tpos = S.op('pe', lambda e: e.matmul(bank[:, 32:64], lhsT=ones_b[:], rhs=ind[:], start=True, stop=True))
                        indb.done(ii, [tpos])
                        V_(lambda e: e.reciprocal(out=ggate, in_=gsum), deps=[tg2])
                        V_(lambda e: e.tensor_scalar(out=w2_, in0=e21, scalar1=1.0, scalar2=None, op0=ALU.add), deps=[te21])
                        V_(lambda e: e.reciprocal(out=w1_, in_=w2_))
                        V_(lambda e: e.tensor_tensor(out=w2_, in0=e21, in1=w1_, op=ALU.mult))
                        V_(lambda e: e.tensor_tensor(out=gates_f[:, t:t + 1], in0=w1_, in1=ggate, op=ALU.mult))
                        V_(lambda e: e.tensor_tensor(out=gates_f[:, 16 + t:17 + t], in0=w2_, in1=ggate, op=ALU.mult))
                        V_(lambda e: e.tensor_tensor(out=posf, in0=bank[:, 0:32], in1=cum[:], op=ALU.add), deps=[tpos])
                        tcum = V_(lambda e: e.tensor_tensor(out=cum[:], in0=bank[:, 32:64], in1=cum[:], op=ALU.add))
                        psr.done(bi, [tcum])
                        V_(lambda e: e.tensor_tensor(out=slotv, in0=posf, in1=eC[:], op=ALU.add))
                        tsl_ = []
                        for k_, oh in enumerate((oh1, oh2)):
                            V_(lambda e: e.scalar_tensor_tensor(out=prod, in0=oh, scalar=1.0, in1=slotv, op0=ALU.mult, op1=ALU.mult, accum_out=s1))
                            V_(lambda e: e.scalar_tensor_tensor(out=prod, in0=oh, scalar=1.0, in1=posf, op0=ALU.mult, op1=ALU.mult, accum_out=p1))
                            V_(lambda e: e.tensor_scalar(out=ov, in0=p1, scalar1=float(CAP), scalar2=None, op0=ALU.is_ge))
                            V_(lambda e: e.tensor_tensor(out=tmp1, in0=TRf[:], in1=s1, op=ALU.subtract))
                            V_(lambda e: e.scalar_tensor_tensor(out=tmp1, in0=tmp1, scalar=ov, in1=s1, op0=ALU.mult, op1=ALU.add))
                            tsl_.append(V_(lambda e: e.tensor_copy(out=slots_i[:, k_ * 16 + t:k_ * 16 + t + 1], in_=tmp1)))
                        cum_tok = [tsl_[-1]]
                        tsc = None
                        for k_ in range(2):
                            tsc = S.idma(out=X_disp, out_offset=bass.IndirectOffsetOnAxis(ap=slots_i[:, k_ * 16 + t:k_ * 16 + t + 1], axis=0),
                                         in_=h3b[:], in_offset=None, deps=[tsl_[k_], tcb], stream=f"sc{b3i}",
                                         bounds_check=NROWS - 1, oob_is_err=False)
                        h3br.done(b3i, [tsc])
                        bgc(3 if t < 6 else 0)
                        cum_tok_[0] = cum_tok

                p4s = {}
                for t in range(NT + 1):
                    if t < NT:
                        p4s[t] = p4_h1(t)
                    if t >= 1:
                        p4_h2(t - 1, p4s.pop(t - 1))
                bgc(1000)
                cum_tok = cum_tok_[0]
                if "route_scr" in taps:
                    slf = alloc(nc, pes, "slf", [128, 32], F32)
                    tq_ = S.op('dve', lambda e: e.tensor_copy(out=slf[:], in_=slots_i[:]), deps=cum_tok)
                    S.dma('sp', route_scr[:, 0:2, :], gates_f[:].rearrange("p (k t) -> p k t", k=2), deps=[tq_], stream="c0")
                    S.dma('sp', route_scr[:, 2:4, :], slf[:].rearrange("p (k t) -> p k t", k=2), deps=[tq_], stream="c0")
                S.barrier(skip=())
            if phases <= 5:
                return

            with ExitStack() as pes:
                w13r = alloc(nc, pes, "w13", [128, 2, KC, 512], BF16, 2)
                Xr = alloc(nc, pes, "Xr", [128, D], BF16, 4)
                XTr = alloc(nc, pes, "XT", [128, KC, 256], BF16, 2)
                sr = alloc(nc, pes, "s6", [128, 256], F32, 3)
                aTr = alloc(nc, pes, "aT", [128, 4, 256], BF16, 2)
                yr = alloc(nc, pes, "y6", [128, D], F32, 3)
                zt = alloc(nc, pes, "zt6", [128, D], F32)
                tz = S.op('pool', lambda e: e.memset(zt[:], 0.0))
                S.dma('sp', Y_disp[NEXP * CAP:NEXP * CAP + 128, :], zt[:], deps=[tz], stream="c0")
                flip = 0
                w2r3 = alloc(nc, pes, "w2c", [128, 4, D], BF16, 3)
                flip_ = [0]

                xl = {}

                def loadX(ex_):
                    lst = []
                    for rb in range(2):
                        xi, Xt, xwar = Xr.next()
                        r0 = ex_ * CAP + rb * 128
                        tx = S.dma('pool', Xt[:], X_disp[r0:r0 + 128, :], deps=xwar, stream=f"X{xi}")
                        lst.append((xi, Xt, tx))
                    xl[ex_] = lst

                loadX(0)

                def stA(ex):
                    if ex + 1 < NEXP:
                        loadX(ex + 1)
                    wi_, w13_, wwar_ = w13r.next()
                    w2i_, w2b_, w2war_ = w2r3.next()
                    S.dma('pool', w13_[:, 0], w1b_scr[ex], deps=wwar_, stream=f"w{wi_}")
                    tw13 = S.dma('pool', w13_[:, 1], w3b_scr[ex], stream=f"w{wi_}")
                    tw2 = S.dma('pool', w2b_[:], w2b_scr[ex], deps=w2war_, stream=f"u{w2i_}")
                    xti, XT, xtwar = XTr.next()
                    tcs = []
                    for rb, (xi, Xt, tx) in enumerate(xl.pop(ex)):
                        tks = transpose_tile(Xt, XT[:, :, rb * 128:(rb + 1) * 128], [tx], xtwar)
                        Xr.done(xi, tks)
                        tcs += tks
                    return dict(wi=wi_, w13=w13_, tw13=[tw13], w2i=w2i_, w2b=w2b_, tw2=[tw2], xti=xti, XT=XT, tcs=tcs)

                def stB(ex, st):
                    w13, XT = st["w13"], st["XT"]
                    ai, aT, awar = aTr.next()
                    tm = None
                    tmul = []
                    for fo in range(4):
                        bi, bank, war = psr.next()
                        for half in range(2):
                            for c in range(KC):
                                tm = S.op('pe', lambda e: e.matmul(
                                    bank[:, half * 256:(half + 1) * 256], lhsT=w13[:, half, c, fo * 128:(fo + 1) * 128],
                                    rhs=XT[:, c, :], start=(c == 0), stop=(c == KC - 1)),
                                    deps=st["tcs"] + st["tw13"] + war if (c == 0 and half == 0) else (), sig=(c == KC - 1 and half == 1))
                        si, s_, swar = sr.next()
                        ts = S.op('act', lambda e: e.activation(out=s_[:], in_=bank[:, 0:256], func=AF.Silu), deps=[tm] + swar)
                        tmu = S.op('dve', lambda e: e.tensor_tensor(out=aT[:, fo, :], in0=bank[:, 256:512], in1=s_[:], op=ALU.mult),
                                   deps=[ts, tm] + awar)
                        psr.done(bi, [tmu])
                        sr.done(si, [tmu])
                        tmul.append(tmu)
                    XTr.done(st["xti"], [tm])
                    w13r.done(st["wi"], [tm])
                    st.update(ai=ai, aT=aT, tmul=tmul)

                def stC(ex, st):
                    aT, w2b = st["aT"], st["w2b"]
                    tm2 = None
                    for rb in range(2):
                        yi, yt, ywar = yr.next()
                        tes = []
                        for cb in range(4):
                            bi, bank, war = psr.next()
                            for fo in range(4):
                                tm2 = S.op('pe', lambda e: e.matmul(bank[:], lhsT=aT[:, fo, rb * 128:(rb + 1) * 128],
                                                                    rhs=w2b[:, fo, cb * 512:(cb + 1) * 512], start=(fo == 0), stop=(fo == 3)),
                                           deps=st["tmul"] + st["tw2"] + war if fo == 0 else (), sig=(fo == 3))
                            flip_[0] ^= 1
                            if flip_[0]:
                                te = S.op('act', lambda e: e.activation(out=yt[:, cb * 512:(cb + 1) * 512], in_=bank[:], func=AF.Copy),
                                          deps=[tm2] + ywar)
                            else:
                                te = S.op('dve', lambda e: e.tensor_copy(out=yt[:, cb * 512:(cb + 1) * 512], in_=bank[:]), deps=[tm2] + ywar)
                            psr.done(bi, [te])
                            tes.append(te)
                        r0 = ex * CAP + rb * 128
                        td = S.dma('sp', Y_disp[r0:r0 + 128, :], yt[:], deps=tes, stream=f"y{yi}")
                        yr.done(yi, [td])
                    aTr.done(st["ai"], [tm2])
                    w2r3.done(st["w2i"], [tm2])

                sts = {}
                for step in range(NEXP + 2):
                    if step < NEXP:
                        sts[step] = stA(step)
                    if 0 <= step - 1 < NEXP:
                        stB(step - 1, sts[step - 1])
                    if 0 <= step - 2 < NEXP:
                        stC(step - 2, sts.pop(step - 2))
                S.barrier()
            if phases <= 6:
                return

            with ExitStack() as pes:
                gf_bc = alloc(nc, pes, "gf_bc", [128, D], F32)
                tg = S.dma('sp', gf_bc[:], g_final.partition_broadcast(128), stream="c0")
                xr = alloc(nc, pes, "x7r", [128, D], F32, 2)
                y1r = alloc(nc, pes, "y1r", [128, D], F32, 2)
                y2r = alloc(nc, pes, "y2r", [128, D], F32, 2)
                outr = alloc(nc, pes, "outr", [128, D], F32, 2)
                junk = alloc(nc, pes, "junk7", [128, D], BF16)
                ss7 = alloc(nc, pes, "ss7", [128, 48], F32)
                tout = []
                ld6 = {}

                def load6(t):
                    r0 = t * 128
                    xi, xt, xwar = xr.next()
                    tx = S.dma('sp', xt[:], x2_scr[r0:r0 + 128, :], deps=xwar, stream=f"x{xi}")
                    y1i, y1, y1war = y1r.next()
                    y2i, y2, y2war = y2r.next()
                    tg1 = S.idma(out=y1[:], out_offset=None, in_=Y_disp, in_offset=bass.IndirectOffsetOnAxis(ap=slots_i[:, t:t + 1], axis=0),
                                 deps=y1war, stream=f"g1{y1i}", bounds_check=NROWS - 1, oob_is_err=False)
                    tg2 = S.idma(out=y2[:], out_offset=None, in_=Y_disp, in_offset=bass.IndirectOffsetOnAxis(ap=slots_i[:, 16 + t:17 + t], axis=0),
                                 deps=y2war, stream=f"g2{y2i}", bounds_check=NROWS - 1, oob_is_err=False)
                    ld6[t] = (r0, xi, xt, tx, y1i, y1, y2i, y2, tg1, tg2)

                load6(0)
                for t in range(NT):
                    r0, xi, xt, tx, y1i, y1, y2i, y2, tg1, tg2 = ld6.pop(t)
                    c1 = S.op('dve', lambda e: e.scalar_tensor_tensor(out=xt[:], in0=y1[:], scalar=gates_f[:, t:t + 1], in1=xt[:],
                                                                      op0=ALU.mult, op1=ALU.add), deps=[tx, tg1])
                    c2 = S.op('dve', lambda e: e.scalar_tensor_tensor(out=xt[:], in0=y2[:], scalar=gates_f[:, 16 + t:17 + t], in1=xt[:],
                                                                      op0=ALU.mult, op1=ALU.add), deps=[tg2])
                    y1r.done(y1i, [c1])
                    y2r.done(y2i, [c2])
                    oi, ot, owar = outr.next()
                    t4, t1 = rmsnorm_tile(xt[:], gf_bc[:], ot[:], ss7[:, t:t + 1], ss7[:, 16 + t:17 + t], ss7[:, 32 + t:33 + t], junk,
                                          [c2, tg] + owar)
                    xr.done(xi, [t4, t1])
                    if t + 1 < NT:
                        load6(t + 1)
                    td = S.dma('sp', out[r0:r0 + 128, :], ot[:], deps=[t4], stream=f"o{oi}")
                    outr.done(oi, [td])
                    tout.append(td)
                S.barrier()

    return nc


def _t5_bucket(d):
    d = np.maximum(d, 0)
    df = np.maximum(d, 1).astype(np.float32)
    large = 16 + (np.log(df / np.float32(16)) / np.float32(math.log(2048 / 16)) * np.float32(16)).astype(np.int32)
    large = np.minimum(large, 31)
    return np.where(d < 16, d, large)


def _bias_tables(rel_bias):
    k = np.arange(128)[:, None]
    q = np.arange(128)[None, :]
    dist = np.stack([q + 128 - k, q - k], axis=0)
    bA = rel_bias[_t5_bucket(dist)][..., :16]
    biasA = np.transpose(bA, (1, 3, 0, 2)).reshape(128, 4, 2, 2, 2, 128)
    biasA = np.ascontiguousarray(np.transpose(biasA, (0, 1, 3, 2, 4, 5))).astype(np.float32)
    bBs = []
    for (_w, r) in ((128, 1), (512, 4), (2048, 16)):
        bb = rel_bias[_t5_bucket(dist * r)][..., 16:]
        bBs.append(np.transpose(bb, (1, 3, 0, 2)))
    biasB = np.ascontiguousarray(np.stack(bBs, axis=1)).astype(np.float32)
    okA = (dist >= 0) & (dist <= 127)
    okB = (dist >= 0) & (dist <= 128)
    maskA = np.ascontiguousarray(np.transpose(np.where(okA, 0.0, NEGM), (1, 0, 2))).astype(np.float32)
    maskB = np.ascontiguousarray(np.transpose(np.where(okB, 0.0, NEGM), (1, 0, 2))).astype(np.float32)
    return biasA, biasB, maskA, maskB


def make_in_maps(inputs, phases=99, cores=range(NCORES)):
    x = np.asarray(inputs["x"], dtype=np.float32)
    rel_bias = np.asarray(inputs["rel_bias"], dtype=np.float32)
    biasA, biasB, maskA, maskB = _bias_tables(rel_bias)
    sq = lambda n: np.ascontiguousarray(np.asarray(inputs[n], dtype=np.float32)[0])
    common = dict(g_mix=sq("g_mix")[None, :], w_in=sq("w_in"), biasA=biasA, biasB=biasB, maskA=maskA, maskB=maskB,
                  sinks=np.ascontiguousarray(sq("sinks_a").reshape(4, 2, 2).transpose(0, 2, 1).reshape(1, 16)))
    if phases >= 3:
        common.update(w_a_out=sq("w_a_out"), w_b_out=sq("w_b_out"), w_gate=sq("w_gate"),
                      b_gate=np.ascontiguousarray(sq("b_gate").reshape(32, 128).T))
    if phases >= 4:
        common.update(w_o=sq("w_o"))
    if phases >= 5:
        common.update(g_x=sq("g_x")[None, :], g_mem=sq("g_mem")[None, :], g_moe=sq("g_moe")[None, :],
                      w_xq=sq("w_xq"), w_xk=sq("w_xk"), w_xv=sq("w_xv"), w_xo=sq("w_xo"),
                      w_r=np.ascontiguousarray(np.concatenate([sq("w_rg"), sq("w_re")], axis=1)))
    if phases >= 6:
        common.update(w1=sq("w1"), w3=sq("w3"), w2=sq("w2"))
    if phases >= 7:
        common.update(g_final=np.asarray(inputs["g_final"], dtype=np.float32)[None, :])
    maps = []
    for c in cores:
        b, qd = c // 4, c % 4
        m = dict(common)
        m["xo"] = np.ascontiguousarray(x[b, qd * T:(qd + 1) * T])
        if qd > 0:
            m["xh"] = np.ascontiguousarray(x[b, (qd - 1) * T:qd * T])
            m["hvalid"] = np.ones((128, 64), np.float32)
        else:
            m["xh"] = np.zeros((T, D), np.float32)
            m["hvalid"] = np.zeros((128, 64), np.float32)
        if phases >= 5:
            m["memb"] = np.ascontiguousarray(np.asarray(inputs["mem"], dtype=np.float32)[b])
        maps.append(m)
    return maps


def kernel(**inputs):
    nc = build()
    maps = make_in_maps(inputs)
    res = run_bass_kernel_spmd(nc, maps, core_ids=list(range(NCORES)))
    outs = [r["out"] for r in res.results]
    full = np.stack([np.concatenate(outs[b * 4:(b + 1) * 4], axis=0) for b in range(2)], axis=0)
    return full.astype(np.float32)
```
